# Optimizing a Trainium2 kernel written in Bass

```python
import math
import jax, jax.numpy as jnp
from jax import lax
import numpy as np

D_MODEL = 1024
BATCH = 8
SEQ = 4096
DEPTH = 2

MIX_WIDTH = D_MODEL
GM_WIDTH = MIX_WIDTH // 2
RW_WIDTH = MIX_WIDTH - GM_WIDTH
HEAD_DIM = 64
GM_HEADS = GM_WIDTH // HEAD_DIM
RW_HEADS = RW_WIDTH // HEAD_DIM
CHUNK = 128
DECAY_RANK = 64
AAA_RANK = 64
GATE_RANK = 128
RW_SHIFT_WIDTH = 3 * RW_WIDTH + DECAY_RANK + AAA_RANK + GATE_RANK
IN_WIDTH = 2 * GM_WIDTH + RW_SHIFT_WIDTH
D_FF = 2816
N_EXPERTS = 8
TOP_K = 2
D_FF_EXPERT = 3584
MOE_BLOCK = 128
N_DENSE = (DEPTH + 1) // 2
N_MOE = DEPTH // 2
RMS_EPS = 1e-6
LN_EPS = 1e-5
GN_EPS = 64e-5

kernel_name = "hybrid_gmlp_rwkv7_moe_trunk"


def rmsnorm(x, g):
    xf = x.astype(jnp.float32)
    y = xf * lax.rsqrt(jnp.mean(xf * xf, axis=-1, keepdims=True) + RMS_EPS)
    return (y * g.astype(jnp.float32)).astype(x.dtype)


def head_layernorm(x, w, b, eps):
    h, n = x.shape[-2], x.shape[-1]
    xf = x.astype(jnp.float32)
    mu = jnp.mean(xf, axis=-1, keepdims=True)
    xc = xf - mu
    var = jnp.mean(xc * xc, axis=-1, keepdims=True)
    y = xc * lax.rsqrt(var + eps)
    y = y * w.astype(jnp.float32).reshape(h, n) + b.astype(jnp.float32).reshape(h, n)
    return y.astype(x.dtype)


def token_shift_mix(p, mu):
    prev = jnp.pad(p, ((0, 0), (1, 0), (0, 0)))[:, :-1]
    return p + (prev - p) * mu


def gmlp_group(pu, pv, ln_w, ln_b, ws, bs):
    b, t, _ = pu.shape
    u = jax.nn.gelu(pu, approximate=False)
    v = jax.nn.gelu(pv, approximate=False)
    v = v.reshape(b, t // CHUNK, CHUNK, GM_HEADS, HEAD_DIM)
    v = head_layernorm(v, ln_w, ln_b, LN_EPS)
    causal = jnp.tril(jnp.ones((CHUNK, CHUNK), dtype=bool))
    w_masked = jnp.where(causal[None], ws, jnp.zeros((), ws.dtype))
    mixed = jnp.einsum('hts,bcshn->bcthn', w_masked, v)
    mixed = mixed + bs.T[None, None, :, :, None]
    return u * mixed.reshape(b, t, GM_WIDTH)


def wkv7_scan(r, w, k, v, a, bb):
    b, t, h, n = r.shape
    xs = tuple(jnp.moveaxis(z, 1, 0) for z in (r, w, k, v, a, bb))

    def step(S, inp):
        r_t, w_t, k_t, v_t, a_t, b_t = inp
        sa = jnp.einsum('bhvk,bhk->bhv', S, a_t)
        S = S * w_t[:, :, None, :] + sa[..., None] * b_t[:, :, None, :] + v_t[..., None] * k_t[:, :, None, :]
        y_t = jnp.einsum('bhvk,bhk->bhv', S, r_t)
        return S, y_t

    S0 = jnp.zeros((b, h, n, n), jnp.float32)
    _, ys = lax.scan(step, S0, xs)
    return jnp.moveaxis(ys, 0, 1)


def rwkv7_group(p, mu, w_up, w0, a_up, a0, g_up, k_k, k_a, r_k, lnx_w, lnx_b):
    b, t, _ = p.shape
    dt = p.dtype
    p = token_shift_mix(p, mu)
    i1, i2, i3 = RW_WIDTH, 2 * RW_WIDTH, 3 * RW_WIDTH
    i4, i5 = i3 + DECAY_RANK, i3 + DECAY_RANK + AAA_RANK
    r, k, v = p[..., :i1], p[..., i1:i2], p[..., i2:i3]
    wd, ad, gd = p[..., i3:i4], p[..., i4:i5], p[..., i5:]
    log_w = -jax.nn.softplus(-(w0 + jnp.tanh(wd) @ w_up)) - 0.5
    decay = jnp.exp(-jnp.exp(log_w.astype(jnp.float32)))
    a = jax.nn.sigmoid(a0 + ad @ a_up)
    g = jax.nn.sigmoid(gd) @ g_up
    heads = lambda z: z.reshape(b, t, RW_HEADS, HEAD_DIM).astype(jnp.float32)
    kk = heads(k * k_k)
    kk = kk / jnp.maximum(jnp.sqrt(jnp.sum(kk * kk, axis=-1, keepdims=True)), 1e-12)
    k = k * (1.0 + (a - 1.0) * k_a)
    rh, kh, vh, ah, wh = heads(r), heads(k), heads(v), heads(a), decay.reshape(b, t, RW_HEADS, HEAD_DIM)
    y = wkv7_scan(rh, wh, kh, vh, -kk, kk * ah)
    y = head_layernorm(y, lnx_w, lnx_b, GN_EPS)
    bonus = jnp.sum(rh * kh * r_k.astype(jnp.float32), axis=-1, keepdims=True) * vh
    y = (y + bonus).reshape(b, t, RW_WIDTH).astype(dt)
    return y * g


def hybrid_mixer(h, w_in, w_out, shift_mu, gm_ln_w, gm_ln_b, gm_ws, gm_bs,
                 rw_w_up, rw_w0, rw_a_up, rw_a0, rw_g_up, rw_k_k, rw_k_a, rw_r_k, rw_lnx_w, rw_lnx_b):
    p = h @ w_in
    y_gm = gmlp_group(p[..., :GM_WIDTH], p[..., GM_WIDTH:2 * GM_WIDTH], gm_ln_w, gm_ln_b, gm_ws, gm_bs)
    y_rw = rwkv7_group(p[..., 2 * GM_WIDTH:], shift_mu, rw_w_up, rw_w0, rw_a_up, rw_a0, rw_g_up,
                       rw_k_k, rw_k_a, rw_r_k, rw_lnx_w, rw_lnx_b)
    y = jnp.concatenate([y_gm, y_rw], axis=-1)
    return y @ w_out


def swiglu(h, wg, wu, wd):
    return (jax.nn.silu(h @ wg) * (h @ wu)) @ wd


def moe_swiglu(xf, router, wg, wu, wd):
    n, d = xf.shape
    logits = (xf @ router).astype(jnp.float32)
    top_val, top_idx = lax.top_k(logits, TOP_K)
    gate = jax.nn.softmax(top_val, axis=-1)
    n_assign = n * TOP_K
    n_rows = ((n_assign + MOE_BLOCK - 1) // MOE_BLOCK + N_EXPERTS) * MOE_BLOCK
    flat_e = top_idx.reshape(-1).astype(jnp.int32)
    flat_tok = jnp.repeat(jnp.arange(n, dtype=jnp.int32), TOP_K)
    flat_gate = gate.reshape(-1)
    order = jnp.argsort(flat_e, stable=True)
    e_sorted = flat_e[order]
    counts = jnp.bincount(flat_e, length=N_EXPERTS)
    padded = (counts + MOE_BLOCK - 1) // MOE_BLOCK * MOE_BLOCK
    pad_end = jnp.cumsum(padded)
    pad_start = pad_end - padded
    grp_start = jnp.cumsum(counts) - counts
    rank = jnp.arange(n_assign, dtype=jnp.int32) - grp_start[e_sorted]
    dest = pad_start[e_sorted] + rank
    row_tok = jnp.full((n_rows,), n, jnp.int32).at[dest].set(flat_tok[order])
    row_gate = jnp.zeros((n_rows,), xf.dtype).at[dest].set(flat_gate[order].astype(xf.dtype))
    block_start = jnp.arange(n_rows // MOE_BLOCK, dtype=jnp.int32) * MOE_BLOCK
    block_expert = jnp.minimum(jnp.searchsorted(pad_end, block_start, side='right'), N_EXPERTS - 1)
    x_pad = jnp.concatenate([xf, jnp.zeros((1, d), xf.dtype)], axis=0)
    xb = x_pad[row_tok].reshape(-1, MOE_BLOCK, d)

    def expert_block(args):
        xblk, e = args
        return (jax.nn.silu(xblk @ wg[e]) * (xblk @ wu[e])) @ wd[e]

    ys = lax.map(expert_block, (xb, block_expert)).reshape(n_rows, d) * row_gate[:, None]
    return jnp.zeros_like(x_pad).at[row_tok].add(ys)[:n]


def setup_inputs(seed: int = 0) -> dict:
    key = jax.random.key(seed)
    ks = jax.random.split(key, 32)
    f32 = jnp.float32
    nrm = lambda k, shape, s: jax.random.normal(k, shape, f32) * s
    L = DEPTH
    return {
        "x": nrm(ks[0], (BATCH, SEQ, D_MODEL), 1.0),
        "norm_mix": 1.0 + nrm(ks[1], (L, D_MODEL), 0.1),
        "w_in": nrm(ks[2], (L, D_MODEL, IN_WIDTH), D_MODEL ** -0.5),
        "w_out": nrm(ks[3], (L, MIX_WIDTH, D_MODEL), MIX_WIDTH ** -0.5),
        "shift_mu": jax.random.uniform(ks[4], (L, RW_SHIFT_WIDTH), f32),
        "gm_ln_w": 1.0 + nrm(ks[5], (L, GM_WIDTH), 0.1),
        "gm_ln_b": nrm(ks[6], (L, GM_WIDTH), 0.1),
        "gm_ws": nrm(ks[7], (L, GM_HEADS, CHUNK, CHUNK), CHUNK ** -0.5),
        "gm_bs": 1.0 + nrm(ks[8], (L, GM_HEADS, CHUNK), 0.1),
        "rw_w_up": nrm(ks[9], (L, DECAY_RANK, RW_WIDTH), 0.1),
        "rw_w0": jax.random.uniform(ks[10], (L, RW_WIDTH), f32, -6.0, -1.0),
        "rw_a_up": nrm(ks[11], (L, AAA_RANK, RW_WIDTH), AAA_RANK ** -0.5),
        "rw_a0": nrm(ks[12], (L, RW_WIDTH), 0.1),
        "rw_g_up": nrm(ks[13], (L, GATE_RANK, RW_WIDTH), GATE_RANK ** -0.5),
        "rw_k_k": 0.85 + nrm(ks[14], (L, RW_WIDTH), 0.1),
        "rw_k_a": 1.0 + nrm(ks[15], (L, RW_WIDTH), 0.1),
        "rw_r_k": nrm(ks[16], (L, RW_HEADS, HEAD_DIM), 0.5),
        "rw_lnx_w": 1.0 + nrm(ks[17], (L, RW_WIDTH), 0.1),
        "rw_lnx_b": nrm(ks[18], (L, RW_WIDTH), 0.1),
        "norm_ffn": 1.0 + nrm(ks[19], (L, D_MODEL), 0.1),
        "ffn_w_gate": nrm(ks[20], (N_DENSE, D_MODEL, D_FF), D_MODEL ** -0.5),
        "ffn_w_up": nrm(ks[21], (N_DENSE, D_MODEL, D_FF), D_MODEL ** -0.5),
        "ffn_w_down": nrm(ks[22], (N_DENSE, D_FF, D_MODEL), D_FF ** -0.5),
        "moe_router": nrm(ks[23], (N_MOE, D_MODEL, N_EXPERTS), D_MODEL ** -0.5),
        "moe_w_gate": nrm(ks[24], (N_MOE, N_EXPERTS, D_MODEL, D_FF_EXPERT), D_MODEL ** -0.5),
        "moe_w_up": nrm(ks[25], (N_MOE, N_EXPERTS, D_MODEL, D_FF_EXPERT), D_MODEL ** -0.5),
        "moe_w_down": nrm(ks[26], (N_MOE, N_EXPERTS, D_FF_EXPERT, D_MODEL), D_FF_EXPERT ** -0.5),
        "norm_final": 1.0 + nrm(ks[27], (D_MODEL,), 0.1),
    }


def reference(x, norm_mix, w_in, w_out, shift_mu, gm_ln_w, gm_ln_b, gm_ws, gm_bs,
              rw_w_up, rw_w0, rw_a_up, rw_a0, rw_g_up, rw_k_k, rw_k_a, rw_r_k, rw_lnx_w, rw_lnx_b,
              norm_ffn, ffn_w_gate, ffn_w_up, ffn_w_down,
              moe_router, moe_w_gate, moe_w_up, moe_w_down, norm_final):
    b, t, d = x.shape
    for i in range(DEPTH):
        h = rmsnorm(x, norm_mix[i])
        x = x + hybrid_mixer(h, w_in[i], w_out[i], shift_mu[i], gm_ln_w[i], gm_ln_b[i], gm_ws[i], gm_bs[i],
                             rw_w_up[i], rw_w0[i], rw_a_up[i], rw_a0[i], rw_g_up[i], rw_k_k[i], rw_k_a[i],
                             rw_r_k[i], rw_lnx_w[i], rw_lnx_b[i])
        h = rmsnorm(x, norm_ffn[i])
        j = i // 2
        if i % 2 == 0:
            x = x + swiglu(h, ffn_w_gate[j], ffn_w_up[j], ffn_w_down[j])
        else:
            y = moe_swiglu(h.reshape(b * t, d), moe_router[j], moe_w_gate[j], moe_w_up[j], moe_w_down[j])
            x = x + y.reshape(b, t, d)
    return rmsnorm(x, norm_final)
```

```python
import numpy as np
import concourse.bass as bass
import concourse.mybir as mybir
from concourse.bass_utils import run_bass_kernel_spmd
from contextlib import ExitStack

F32 = mybir.dt.float32
BF16 = mybir.dt.bfloat16
AF = mybir.ActivationFunctionType
ALU = mybir.AluOpType
AX = mybir.AxisListType

ENGS = ("pe", "act", "dve", "pool", "sp")


class Lane:
    def __init__(self, name, wait_all=False):
        self.name = name
        self.count = 0
        self.wait_all = wait_all
        self.last = None


class Op:
    __slots__ = ("eng", "emit", "deps", "lane", "lane_count", "signaled", "seq",
                 "eidx", "snap", "waits")


def _box(ap):
    t = ap.tensor
    esz = mybir.dt.size(t.dtype)
    name = t.name
    off = int(ap.offset)
    dims = [(int(s), int(c)) for (s, c) in ap.ap]
    if "DRam" in type(t).__name__:
        ext = 0
        for s, c in dims:
            ext += abs(s) * (c - 1)
        return (name, 0, 1, off * esz, (off + ext + 1) * esz)
    pstride = 1
    for d in list(t.shape)[1:]:
        pstride *= int(d)
    p0 = off // pstride
    f0 = off % pstride
    pext = 0
    fext = 0
    for i, (s, c) in enumerate(dims):
        if i == 0 and s != 0 and s % pstride == 0:
            pext += (s // pstride) * (c - 1)
        else:
            fext += abs(s) * (c - 1)
    if "PSum" in type(t).__name__:
        return (name, (p0 // 32) * 32, ((p0 + pext) // 32 + 1) * 32, 0, 2048)
    return (name, p0, p0 + pext + 1, f0 * esz, (f0 + fext + 1) * esz)


def _overlap(a, b):
    return a[1] < b[2] and b[1] < a[2] and a[3] < b[4] and b[3] < a[4]


def _covers(a, b):
    return a[1] <= b[1] and a[2] >= b[2] and a[3] <= b[3] and a[4] >= b[4]


class Prog:
    def __init__(self, nc):
        self.nc = nc
        self.ops = []
        self.eng_ops = {e: [] for e in ENGS}
        self.acc = {}
        self.lanes = []

    def lane(self, name, wait_all=False):
        l = Lane(name, wait_all)
        self.lanes.append(l)
        return l

    def add(self, eng, emit, reads=(), writes=(), lane=None, extra_deps=()):
        op = Op()
        op.eng = eng
        op.emit = emit
        op.lane = lane
        op.signaled = lane is not None
        op.seq = None
        op.snap = None
        op.waits = None
        if lane is not None:
            lane.count += 1
            op.lane_count = lane.count
            lane.last = op
        else:
            op.lane_count = 0
        deps = set(extra_deps)
        key = lane.name if lane is not None else eng
        rboxes = [_box(ap) for ap in reads]
        wboxes = [_box(ap) for ap in writes]
        wboxes += [b for b in rboxes if b[0].startswith("pb")]
        rboxes = [b for b in rboxes if not b[0].startswith("pb")]
        for b in rboxes:
            rec = self.acc.get(b[0])
            if rec is None:
                rec = self.acc[b[0]] = {"w": [], "r": []}
            for (wb, wop, wk) in rec["w"]:
                if _overlap(wb, b):
                    deps.add(wop)
        for b in wboxes:
            rec = self.acc.get(b[0])
            if rec is None:
                rec = self.acc[b[0]] = {"w": [], "r": []}
            for (wb, wop, wk) in rec["w"]:
                if _overlap(wb, b):
                    deps.add(wop)
            for (rb, rop, rk) in rec["r"]:
                if _overlap(rb, b):
                    deps.add(rop)
        for b in rboxes:
            rec = self.acc[b[0]]
            rec["r"] = [(rb, rop, rk) for (rb, rop, rk) in rec["r"]
                        if not (rk == key and _covers(b, rb))]
            rec["r"].append((b, op, key))
        for b in wboxes:
            rec = self.acc[b[0]]
            rec["w"] = [(wb, wop, wk) for (wb, wop, wk) in rec["w"] if not _covers(b, wb)]
            rec["r"] = [(rb, rop, rk) for (rb, rop, rk) in rec["r"] if not _covers(b, rb)]
            rec["w"].append((b, op, key))
        deps.discard(op)
        op.deps = deps
        op.eidx = len(self.eng_ops[eng])
        self.eng_ops[eng].append(op)
        self.ops.append(op)
        return op

    def barrier(self):
        lasts = []
        for e in ENGS:
            if self.eng_ops[e]:
                lasts.append(self.eng_ops[e][-1])
        for l in self.lanes:
            if l.last is not None:
                lasts.append(l.last)
        for e in ENGS:
            self.add(e, lambda eng: eng.nop(), extra_deps=lasts)
        self.acc = {}

    def _skip(self, op, d, win):
        if d.lane is not None:
            return False
        if d.eng != op.eng:
            return False
        if d.eng == "pe":
            return True
        if op.lane is None and op.eidx - d.eidx > win:
            return True
        return False

    def finalize(self, same_eng_window=3):
        for op in self.ops:
            for d in op.deps:
                if d.lane is None and not self._skip(op, d, same_eng_window):
                    d.signaled = True
        cnt = {e: 0 for e in ENGS}
        for op in self.ops:
            if op.lane is None and op.signaled:
                cnt[op.eng] += 1
                op.seq = cnt[op.eng]
        clock = {e: {} for e in ENGS}
        nwaits = 0
        for op in self.ops:
            ck = clock[op.eng]
            need = {}
            for d in op.deps:
                if self._skip(op, d, same_eng_window):
                    continue
                if d.lane is not None:
                    k = ("L", d.lane.name)
                    v = (d.lane.count if d.lane.wait_all else d.lane_count) * 16
                else:
                    k = ("E", d.eng)
                    v = d.seq
                if ck.get(k, 0) >= v:
                    continue
                if need.get(k, (0, None))[0] < v:
                    need[k] = (v, d)
            waits = []
            for k, (v, d) in need.items():
                if ck.get(k, 0) >= v:
                    continue
                waits.append((k, v))
                ck[k] = v
                if d.snap is not None:
                    for kk, vv in d.snap.items():
                        if ck.get(kk, 0) < vv:
                            ck[kk] = vv
            op.waits = waits
            nwaits += len(waits)
            if op.signaled:
                op.snap = dict(ck)
                if op.lane is None:
                    op.snap[("E", op.eng)] = max(op.snap.get(("E", op.eng), 0), op.seq)
        self.nwaits = nwaits

    def emit(self, es):
        nc = self.nc
        sems = {}
        for e in ENGS:
            sems[("E", e)] = es.enter_context(nc.semaphore("s_" + e))
        for l in self.lanes:
            if l.count > 0:
                sems[("L", l.name)] = es.enter_context(nc.semaphore("l_" + l.name))
        block = es.enter_context(nc.Block())

        def run(eng_name, e):
            for op in self.eng_ops[eng_name]:
                for (k, v) in op.waits:
                    e.wait_ge(sems[k], v)
                ins = op.emit(e)
                if op.lane is not None:
                    ins.then_inc(sems[("L", op.lane.name)], 16)
                elif op.signaled:
                    ins.then_inc(sems[("E", eng_name)], 1)

        @block.tensor
        def _(e):
            run("pe", e)

        @block.scalar
        def _(e):
            run("act", e)

        @block.vector
        def _(e):
            run("dve", e)

        @block.gpsimd
        def _(e):
            run("pool", e)

        @block.sync
        def _(e):
            run("sp", e)


T = 4096
D = 1024
NCH = T // 128
INW = 2816
DFF = 2816
NE = 8
DFE = 3584
CDEC = -float(np.exp(-0.5))
RMS_EPS = 1e-6
LN_EPS = 1e-5
GN_EPS = 64e-5

C_ID = 0
C_M4 = 128
C_MQ = 640
C_UT = 768
C_LT = 1024
C_BO = 1152
C_TM = 1280
C_I2 = 1282
CW = 1284
K_GMIX = 0
K_GFFN = 8
K_MU = 16
K_A0 = 30
K_KK = 34
K_KA = 38
K_RK = 42
KW = 46


def make_consts():
    c = np.zeros((128, CW), np.float32)
    s = np.arange(128)[:, None]
    t = np.arange(128)[None, :]
    c[:, C_ID:C_ID + 128] = (s == t)
    strict = (s < t).astype(np.float32)
    incl = (s <= t).astype(np.float32)
    c[:, C_M4:C_M4 + 512] = np.concatenate([strict, incl, strict, incl], 1)
    c[:, C_MQ:C_MQ + 128] = (t < s)
    mid = (s <= 63).astype(np.float32)
    c[:, C_UT:C_UT + 128] = CDEC * (incl - mid)
    c[:, C_UT + 128:C_UT + 256] = CDEC * (strict - mid)
    c[:, C_TM] = CDEC
    c[:, C_TM + 1] = CDEC * mid[:, 0]
    c[:, C_LT:C_LT + 128] = CDEC * (s > t)
    c[:, C_BO:C_BO + 128] = ((s // 64) == (t // 64))
    c[:, C_I2] = (s[:, 0] < 64)
    c[:, C_I2 + 1] = (s[:, 0] >= 64)
    return c


class _Cut(Exception):
    pass


class KB:
    def __init__(self, debug=False, stop_after=None, nch=NCH):
        self.nch = nch
        import os as _os
        self.cut = int(_os.environ.get('KCUT', '99'))
        self.debug = debug
        self.stop_after = stop_after
        self.nc = bass.Bass("TRN2", target_bir_lowering=False)
        self.P = Prog(self.nc)
        self.uid = 0

    def tt(self, out, in0, in1, op, eng="dve"):
        self.P.add(eng, lambda e: e.tensor_tensor(out=out, in0=in0, in1=in1, op=op), reads=[in0, in1], writes=[out])

    def ts(self, out, in0, s1, s2, op0, op1=None, eng="dve"):
        reads = [in0]
        if not isinstance(s1, (int, float)):
            reads.append(s1)
        if s2 is not None and not isinstance(s2, (int, float)):
            reads.append(s2)
        if op1 is None:
            self.P.add(eng, lambda e: e.tensor_scalar(out=out, in0=in0, scalar1=s1, scalar2=None, op0=op0),
                       reads=reads, writes=[out])
        else:
            self.P.add(eng, lambda e: e.tensor_scalar(out=out, in0=in0, scalar1=s1, scalar2=s2, op0=op0, op1=op1),
                       reads=reads, writes=[out])

    def stt(self, out, in0, scalar, in1, op0, op1):
        reads = [in0, in1]
        if not isinstance(scalar, (int, float)):
            reads.append(scalar)
        self.P.add("dve", lambda e: e.scalar_tensor_tensor(out=out, in0=in0, scalar=scalar, in1=in1, op0=op0, op1=op1),
                   reads=reads, writes=[out])

    def cp(self, out, in_, eng="dve"):
        if eng == "act":
            self.P.add("act", lambda e: e.copy(out=out, in_=in_), reads=[in_], writes=[out])
        else:
            self.P.add(eng, lambda e: e.tensor_copy(out=out, in_=in_), reads=[in_], writes=[out])

    def red(self, out, in_, op=ALU.add):
        self.P.add("dve", lambda e: e.tensor_reduce(out=out, in_=in_, axis=AX.X, op=op), reads=[in_], writes=[out])

    def recip(self, out, in_):
        self.P.add("dve", lambda e: e.reciprocal(out=out, in_=in_), reads=[in_], writes=[out])

    def act(self, out, in_, func, bias=None, scale=None, accum_out=None):
        reads = [in_]
        writes = [out]
        kw = {}
        if bias is not None:
            kw["bias"] = bias
            if not isinstance(bias, (int, float)):
                reads.append(bias)
        if scale is not None:
            kw["scale"] = scale
            if not isinstance(scale, (int, float)):
                reads.append(scale)
        if accum_out is not None:
            kw["accum_out"] = accum_out
            writes.append(accum_out)
        self.P.add("act", lambda e: e.activation(out=out, in_=in_, func=func, **kw), reads=reads, writes=writes)

    def mm(self, out, lhsT, rhs, start=True, stop=True):
        self.P.add("pe", lambda e: e.matmul(out, lhsT=lhsT, rhs=rhs, start=start, stop=stop),
                   reads=[lhsT, rhs], writes=[out])

    def tr(self, out, in_, ident):
        self.P.add("pe", lambda e: e.transpose(out=out, in_=in_, identity=ident), reads=[in_, ident], writes=[out])

    def dma(self, out, in_, lane, eng="sp"):
        self.P.add(eng, lambda e: e.dma_start(out=out, in_=in_), reads=[in_], writes=[out], lane=lane)

    def memset(self, ap, val, eng="pool"):
        self.P.add(eng, lambda e: e.memset(ap, val), writes=[ap])

    def bank(self):
        b = self.banks[self.bank_i % 8]
        self.bank_i += 1
        return b

    def bank_bf(self):
        return self.bank().bitcast(BF16)

    def build(self):
        nc = self.nc
        P = self.P
        dbg = self.debug
        din = lambda name, shape: nc.dram_tensor(name, shape, F32, kind="ExternalInput").ap()
        self.x = din("x", [T, D])
        self.consts_d = din("consts", [128, CW])
        self.cols_d = din("cols", [2, 128, KW])
        self.rows_d = din("rows", [2, 5, 512])
        self.bsT_d = din("bsT", [2, 128, 8])
        self.wsT_d = din("wsT", [2, 128, 8, 128])
        self.nfin_d = din("norm_final", [D])
        self.w_in_d = din("w_in", [2, D, INW])
        self.w_out_d = din("w_out", [2, D, D])
        self.w_up_d = din("rw_w_up", [2, 64, 512])
        self.a_up_d = din("rw_a_up", [2, 64, 512])
        self.g_up_d = din("rw_g_up", [2, 128, 512])
        self.fg_d = din("ffn_w_gate", [1, D, DFF])
        self.fu_d = din("ffn_w_up", [1, D, DFF])
        self.fd_d = din("ffn_w_down", [1, DFF, D])
        self.rt_d = din("moe_router_l", [128, 8, NE])
        self.mg_d = din("moe_w_gate", [1, NE, D, DFE])
        self.mu_d = din("moe_w_up", [1, NE, D, DFE])
        self.md_d = din("moe_w_down", [1, NE, DFE, D])
        self.y = nc.dram_tensor("y", [T, D], F32, kind="ExternalOutput").ap()
        skind = "ExternalOutput" if dbg else "Internal"
        self.xa = nc.dram_tensor("xa", [T, D], F32, kind=skind).ap()
        self.xb = nc.dram_tensor("xb", [T, D], F32, kind=skind).ap()
        self.xc = nc.dram_tensor("xc", [T, D], F32, kind=skind).ap()

        with ExitStack() as es:
            self.banks = [es.enter_context(nc.psum_tensor("pb%d" % i, [128, 512], F32)) for i in range(8)]
            self.bank_i = 0
            sb = lambda name, shape, dt: es.enter_context(nc.sbuf_tensor(name, shape, dt))
            self.cst = sb("cst", [128, CW], F32)
            self.idb = sb("idb", [128, 128], BF16)
            self.i2b = sb("i2b", [128, 2], BF16)
            self.lc = P.lane("const", wait_all=True)
            self.dma(self.cst[:], self.consts_d, self.lc)
            self.cp(self.idb[:], self.cst[:, C_ID:C_ID + 128])
            self.cp(self.i2b[:], self.cst[:, C_I2:C_I2 + 2])
            self.lane_x = [P.lane("x0"), P.lane("x1")]
            self.lane_o = [P.lane("o0"), P.lane("o1")]
            self.lane_w = P.lane("w", wait_all=False)

            stages = [("mix0", lambda: self.mixer(0, self.x, self.xa)),
                      ("ffn0", lambda: self.ffn_dense(self.xa, self.xb)),
                      ("mix1", lambda: self.mixer(1, self.xb, self.xc)),
                      ("moe", lambda: self.moe(self.xc, self.y))]
            import os as _os
            only = _os.environ.get("KSTAGES")
            for name, fn in stages:
                if only is not None and name not in only.split(","):
                    src, dst = {"mix0": (self.x, self.xa), "ffn0": (self.xa, self.xb), "mix1": (self.xb, self.xc),
                                "moe": (self.xc, self.y)}[name]
                    cp_lane = P.lane("cp_" + name, wait_all=True)
                    for ci in range(32):
                        self.dma(dst[ci * 128:(ci + 1) * 128, :], src[ci * 128:(ci + 1) * 128, :], cp_lane)
                else:
                    fn()
                P.barrier()
                if self.stop_after == name:
                    break
            outs = [self.y] + ([self.xa, self.xb, self.xc] if dbg else [])
            P.add("sp", lambda e: e.nop(), reads=outs)
            P.finalize()
            print("ops", len(P.ops), "waits", P.nwaits, {e: len(P.eng_ops[e]) for e in ENGS})
            P.emit(es)
        return nc

    def norm_T(self, xt, gcol, hT, tmp, hT32=None):
        xs, ssq, rstd = tmp["xs"], tmp["ssq"], tmp["rstd"]
        self.act(xs, xt, AF.Square, accum_out=ssq)
        self.act(rstd, ssq, AF.Sqrt, scale=1.0 / D, bias=RMS_EPS)
        self.recip(rstd, rstd)
        self.act(xs, xt, AF.Copy, scale=rstd)
        idf = self.cst[:, C_ID:C_ID + 128]
        for half in range(2):
            pb = self.bank()
            pv = pb[:].rearrange("p (a b) -> p a b", a=4)
            for c in range(4):
                cc = half * 4 + c
                self.tr(pv[:, c, :], xs[:, cc * 128:(cc + 1) * 128], idf)
            g = gcol[:, half * 4:(half + 1) * 4].unsqueeze(2).broadcast_to([128, 4, 128])
            self.tt(hT[:, half * 4:(half + 1) * 4, :], pv, g, ALU.mult)
            if hT32 is not None:
                self.cp(hT32[:, half * 4:(half + 1) * 4, :], pv, eng="act")

    def mixer(self, l, xin, xout):
        nc = self.nc
        P = self.P
        with ExitStack() as es:
            cnt = [0]

            def sb(shape, dt=F32, name=None):
                cnt[0] += 1
                return es.enter_context(nc.sbuf_tensor("m%d_%s%d" % (l, name or "t", cnt[0]), shape, dt))

            dbl = lambda shape, dt=F32, name=None: [sb(shape, dt, name), sb(shape, dt, name)]
            def sgl(shape, dt=F32, name=None):
                t_ = sb(shape, dt, name)
                return [t_, t_]
            w_in = sb([128, 8, INW], BF16, "win")
            w_out = sb([128, 8, D], BF16, "wout")
            w_up = sb([64, 512], BF16, "wup")
            a_up = sb([128, 512], BF16, "aup")
            g_up = sb([128, 512], BF16, "gup")
            cols = sb([128, KW], F32, "cols")
            omka = sb([128, 4], F32, "omka")
            rows = sb([128, 5, 512], F32, "rows")
            bsT = sb([128, 8], F32, "bsT")
            wsT32 = sb([128, 8, 128], F32, "wsT32")
            wsT = sb([128, 8, 128], BF16, "wsT")
            lw = P.lane("mw%d" % l, wait_all=True)
            lws = P.lane("mws%d" % l, wait_all=True)
            for c in range(8):
                self.dma(w_in[:, c, :], self.w_in_d[l, c * 128:(c + 1) * 128, :], lw, eng="pool")
            self.dma(w_out[:], self.w_out_d[l].rearrange("(c p) n -> p c n", p=128), lw, eng="pool")
            self.dma(w_up[:], self.w_up_d[l], lw, eng="pool")
            self.dma(a_up[64:128, :], self.a_up_d[l], lw, eng="pool")
            self.dma(g_up[:], self.g_up_d[l], lw, eng="pool")
            self.dma(cols[:], self.cols_d[l], lws)
            for i in range(5):
                self.dma(rows[:, i:i + 1, :], self.rows_d[l, i:i + 1, :].partition_broadcast(128), lws)
            self.dma(bsT[:], self.bsT_d[l], lws)
            self.dma(wsT32[:], self.wsT_d[l], lws)
            m_incl = self.cst[:, C_M4 + 128:C_M4 + 256]
            self.tt(wsT[:], wsT32[:], m_incl.unsqueeze(1).broadcast_to([128, 8, 128]), ALU.mult)
            self.ts(omka[:], cols[:, K_KA:K_KA + 4], -1.0, 1.0, ALU.mult, ALU.add)
            w0_bc = rows[:, 0, :]
            glw_bc = rows[:, 1, :]
            glb_bc = rows[:, 2, :]
            lxw_bc = rows[:, 3, :]
            lxb_bc = rows[:, 4, :]
            gcol = cols[:, K_GMIX:K_GMIX + 8]

            xc_ = dbl([128, D], F32, "xc")
            xs_ = sgl([128, D], F32, "xs")
            ssq_ = sgl([128, 1], F32, "ssq")
            rstd_ = sgl([128, 1], F32, "rstd")
            hT_ = dbl([128, 8, 128], BF16, "hT")
            u_ = sgl([128, 512], F32, "u")
            vg_ = sgl([128, 512], F32, "vg")
            sq_ = sgl([128, 512], F32, "sq")
            st_ = sgl([128, 48], F32, "st")
            z_ = sgl([128, 512], BF16, "z")
            tA_ = sgl([128, 512], F32, "tA")
            ycat_ = sgl([128, D], BF16, "ycat")
            pf_ = dbl([128, 14, 129], F32, "pf")
            psf_ = sgl([128, 14, 128], F32, "psf")
            twT_ = sgl([64, 128], BF16, "twT")
            adT_ = sgl([128, 128], BF16, "adT")
            sgT_ = sgl([128, 128], BF16, "sgT")
            sigw_ = sgl([128, 512], F32, "sigw")
            gam_ = sgl([128, 4, 128], F32, "gam")
            igam_ = sgl([128, 4, 128], F32, "igam")
            gamx_ = sgl([128, 4, 128], F32, "gamx")
            erev_ = sgl([128, 512], F32, "erev")
            gtm_ = sgl([128, 4, 2], F32, "gtm")
            alr_ = sgl([128, 4, 128], F32, "alr")
            kkr_ = sgl([128, 512], F32, "kkr")
            kk_ = sgl([128, 512], F32, "kk")
            rn_ = sgl([128, 512], F32, "rn")
            kp_ = sgl([128, 512], F32, "kp")
            bb_ = sgl([128, 512], F32, "bb")
            tmp_ = sgl([128, 512], F32, "tmp")
            arT_ = sgl([128, 4, 2, 128], BF16, "arT")
            ktT_ = sgl([128, 4, 128], BF16, "ktT")
            btT_ = sgl([128, 4, 128], BF16, "btT")
            rk_ = sgl([128, 4, 128], BF16, "rk")
            b16_ = sgl([128, 4, 128], BF16, "b16")
            kp16_ = sgl([128, 4, 128], BF16, "kp16")
            v16_ = sgl([128, 4, 128], BF16, "v16")
            atok_ = sgl([128, 512], BF16, "atok")
            vtok_ = sgl([128, 512], BF16, "vtok")
            bhat_ = sgl([128, 512], BF16, "bhat")
            khat_ = sgl([128, 512], BF16, "khat")
            AT4_ = sgl([128, 8, 512], BF16, "AT4")
            PQ_ = [sb([128, 8, 2, 128], BF16, "PQ") for _ in range(2)]
            TT_ = [sb([128, 8, 128], BF16, "TT") for _ in range(2)]
            WT_ = sgl([128, 4, 128], BF16, "WT")
            AV_ = sgl([128, 512], BF16, "AV")
            U0_ = sgl([128, 512], F32, "U0")
            U_ = sgl([128, 512], BF16, "U")
            H0bd = sb([128, 4, 128], BF16, "H0bd")
            Hst = sb([128, 4, 64], F32, "Hst")
            yv_ = sgl([128, 512], F32, "yv")
            coef_ = sgl([128, 8], F32, "coef")
            yT_ = sgl([128, 8, 128], BF16, "yT")

            self.memset(H0bd[:], 0.0)
            self.memset(Hst[:], 0.0)
            self.memset(pf_[1][:, :, 128:129], 0.0)
            idb = self.idb[:]
            mask4 = self.cst[:, C_M4:C_M4 + 512]
            maskQ = self.cst[:, C_MQ:C_MQ + 128]
            UTall = self.cst[:, C_UT:C_UT + 256]
            TM = self.cst[:, C_TM:C_TM + 2]
            LT = self.cst[:, C_LT:C_LT + 128]
            BO = self.cst[:, C_BO:C_BO + 128]

            def bc3(ap2, n):
                a = ap2.shape[1]
                return ap2.unsqueeze(2).broadcast_to([ap2.shape[0], a, n])

            for ch in range(self.nch):
                try:
                    par = ch % 2
                    xc = xc_[par]
                    st = st_[par]
                    self.dma(xc[:], xin[ch * 128:(ch + 1) * 128, :], self.lane_x[par])
                    hT = hT_[par]
                    self.norm_T(xc[:], gcol, hT[:], {"xs": xs_[par][:], "ssq": ssq_[par][:], "rstd": rstd_[par][:]})
                    if self.cut == 1:
                        raise _Cut()
                    pu = self.bank()
                    pvb = self.bank()
                    for c in range(8):
                        self.mm(pu[:], hT[:, c, :], w_in[:, c, 0:512], start=(c == 0), stop=(c == 7))
                    for c in range(8):
                        self.mm(pvb[:], hT[:, c, :], w_in[:, c, 512:1024], start=(c == 0), stop=(c == 7))
                    u = u_[par]
                    vg = vg_[par]
                    self.act(u[:], pu[:], AF.Gelu)
                    self.act(vg[:], pvb[:], AF.Gelu)
                    if self.cut == 2:
                        raise _Cut()
                    pf = pf_[par]
                    psf = psf_[par]
                    for jb in range(4):
                        nb = 4 if jb < 3 else 2
                        pb = self.bank()
                        pbv = pb[:].rearrange("p (a b) -> p a b", a=4)
                        for jj in range(nb):
                            j = jb * 4 + jj
                            for c in range(8):
                                self.mm(pbv[:, jj, :], w_in[:, c, 1024 + j * 128:1024 + (j + 1) * 128], hT[:, c, :],
                                        start=(c == 0), stop=(c == 7))
                        if jb % 2 == 0:
                            self.cp(pf[:, jb * 4:jb * 4 + nb, 1:129], pbv[:, 0:nb, :], eng="act")
                        else:
                            self.cp(pf[:, jb * 4:jb * 4 + nb, 1:129], pbv[:, 0:nb, :], eng="dve")
                    self.cp(pf[:, :, 0:1], pf_[1 - par][:, :, 128:129], eng="pool")
                    mu_bc = bc3(cols[:, K_MU:K_MU + 14], 128)
                    self.tt(psf[:], pf[:, :, 0:128], pf[:, :, 1:129], ALU.subtract, eng="pool")
                    self.tt(psf[:], psf[:], mu_bc, ALU.mult, eng="pool")
                    self.tt(psf[:], psf[:], pf[:, :, 1:129], ALU.add, eng="pool")
                    rT = psf[:, 0:4, :]
                    kT = psf[:, 4:8, :]
                    vT = psf[:, 8:12, :]
                    if self.cut == 3:
                        raise _Cut()
                    sq = sq_[par]
                    self.red(st[:, 0:8], vg[:].rearrange("p (h n) -> p h n", h=8))
                    self.act(sq[:], vg[:], AF.Square)
                    self.red(st[:, 8:16], sq[:].rearrange("p (h n) -> p h n", h=8))
                    self.ts(st[:, 0:8], st[:, 0:8], 1.0 / 64, None, ALU.mult)
                    self.tt(st[:, 16:24], st[:, 0:8], st[:, 0:8], ALU.mult)
                    self.stt(st[:, 8:16], st[:, 8:16], 1.0 / 64, st[:, 16:24], ALU.mult, ALU.subtract)
                    self.act(st[:, 8:16], st[:, 8:16], AF.Sqrt, bias=LN_EPS)
                    self.recip(st[:, 8:16], st[:, 8:16])
                    tA = tA_[par]
                    v3 = vg[:].rearrange("p (h n) -> p h n", h=8)
                    t3 = tA[:].rearrange("p (h n) -> p h n", h=8)
                    self.tt(t3, v3, bc3(st[:, 0:8], 64), ALU.subtract)
                    self.tt(t3, t3, bc3(st[:, 8:16], 64), ALU.mult)
                    self.tt(tA[:], tA[:], glw_bc, ALU.mult, eng="pool")
                    z = z_[par]
                    self.tt(z[:], tA[:], glb_bc, ALU.add, eng="pool")
                    pm = self.bank()
                    for h in range(8):
                        self.mm(pm[:, h * 64:(h + 1) * 64], wsT[:, h, :], z[:, h * 64:(h + 1) * 64])
                    ycat = ycat_[par]
                    self.tt(t3, pm[:].rearrange("p (h n) -> p h n", h=8), bc3(bsT[:], 64), ALU.add)
                    self.tt(ycat[:, 0:512], tA[:], u[:], ALU.mult)
                    if self.cut == 4:
                        raise _Cut()
                    twT = twT_[par]
                    adT = adT_[par]
                    sgT = sgT_[par]
                    self.act(twT[:], psf[0:64, 12, :], AF.Tanh)
                    self.cp(adT[64:128, :], psf[64:128, 12, :], eng="pool")
                    self.act(sgT[:], psf[:, 13, :], AF.Sigmoid)
                    if self.cut == 41:
                        raise _Cut()
                    pd = self.bank()
                    self.mm(pd[:], twT[:], w_up[:])
                    sigw = sigw_[par]
                    self.tt(sigw[:], pd[:], w0_bc, ALU.add)
                    self.act(sigw[:], sigw[:], AF.Sigmoid)
                    if self.cut == 42:
                        raise _Cut()
                    pc0 = self.bank()
                    pc1 = self.bank()
                    pcs = [pc0, pc1]
                    for j in range(4):
                        self.mm(pcs[j // 2][:, (j % 2) * 256:(j % 2 + 1) * 256], sigw[:, j * 128:(j + 1) * 128], UTall)
                    if self.cut == 43:
                        raise _Cut()
                    ptm = self.bank()
                    for j in range(4):
                        self.mm(ptm[:, j * 2:(j + 1) * 2], sigw[:, j * 128:(j + 1) * 128], TM)
                    if self.cut == 44:
                        raise _Cut()
                    prv = self.bank()
                    self.mm(prv[:, 0:256], LT, sigw[:, 0:256])
                    self.mm(prv[:, 256:512], LT, sigw[:, 256:512])
                    if self.cut == 45:
                        raise _Cut()
                    gam = gam_[par]
                    igam = igam_[par]
                    gamx = gamx_[par]
                    erev = erev_[par]
                    gtm = gtm_[par]
                    for hb in range(2):
                        pcv = pcs[hb][:].rearrange("p (j w t) -> p j w t", j=2, w=2)
                        self.act(gam[:, hb * 2:(hb + 1) * 2, :], pcv[:, :, 0, :], AF.Exp)
                        self.act(igam[:, hb * 2:(hb + 1) * 2, :], pcv[:, :, 0, :], AF.Exp, scale=-1.0)
                        self.act(gamx[:, hb * 2:(hb + 1) * 2, :], pcv[:, :, 1, :], AF.Exp)
                    self.act(erev[:], prv[:], AF.Exp)
                    self.act(gtm[:].rearrange("p j w -> p (j w)"), ptm[:, 0:8], AF.Exp)
                    if self.cut == 5:
                        raise _Cut()
                    pa = self.bank()
                    pav = pa[:].rearrange("p (a b) -> p a b", a=4)
                    for j in range(4):
                        self.mm(pav[:, j, :], a_up[64:128, j * 128:(j + 1) * 128], adT[64:128, :])
                    alr = alr_[par]
                    for j in range(4):
                        self.act(alr[:, j, :], pav[:, j, :], AF.Sigmoid, bias=cols[:, K_A0 + j:K_A0 + j + 1])
                    kkr = kkr_[par]
                    kk = kk_[par]
                    rn = rn_[par]
                    kkr3 = kkr[:].rearrange("p (j t) -> p j t", j=4)
                    kk3 = kk[:].rearrange("p (j t) -> p j t", j=4)
                    self.tt(kkr3, kT, bc3(cols[:, K_KK:K_KK + 4], 128), ALU.mult)
                    self.tt(rn[:], kkr[:], kkr[:], ALU.mult, eng="pool")
                    pn = self.bank()
                    self.mm(pn[:, 0:256], BO, rn[:, 0:256])
                    self.mm(pn[:, 256:512], BO, rn[:, 256:512])
                    self.ts(rn[:], pn[:], 1e-18, None, ALU.max)
                    self.act(rn[:], rn[:], AF.Ln)
                    self.act(rn[:], rn[:], AF.Exp, scale=-0.5)
                    self.tt(kk[:], kkr[:], rn[:], ALU.mult)
                    tmp = tmp_[par]
                    tmp3 = tmp[:].rearrange("p (j t) -> p j t", j=4)
                    for j in range(4):
                        self.ts(tmp3[:, j, :], alr[:, j, :], cols[:, K_KA + j:K_KA + j + 1], omka[:, j:j + 1],
                                ALU.mult, ALU.add)
                    kp = kp_[par]
                    bb = bb_[par]
                    kp3 = kp[:].rearrange("p (j t) -> p j t", j=4)
                    bb3 = bb[:].rearrange("p (j t) -> p j t", j=4)
                    self.tt(kp3, kT, tmp3, ALU.mult)
                    self.tt(bb3, kk3, alr[:], ALU.mult, eng="pool")
                    arT = arT_[par]
                    ktT = ktT_[par]
                    btT = btT_[par]
                    rk = rk_[par]
                    self.tt(arT[:, :, 1, :], rT, gam[:], ALU.mult)
                    self.stt(arT[:, :, 0, :], kk3, -1.0, gamx[:], ALU.mult, ALU.mult)
                    self.tt(ktT[:], kp3, igam[:], ALU.mult)
                    self.tt(btT[:], bb3, igam[:], ALU.mult, eng="pool")
                    for j in range(4):
                        self.stt(rk[:, j, :], psf[:, j, :], cols[:, K_RK + j:K_RK + j + 1], kp3[:, j, :], ALU.mult, ALU.mult)
                    b16 = b16_[par]
                    kp16 = kp16_[par]
                    v16 = v16_[par]
                    self.cp(b16[:], bb3, eng="pool")
                    self.cp(kp16[:], kp3, eng="pool")
                    self.cp(v16[:], vT, eng="act")
                    if self.cut == 6:
                        raise _Cut()
                    ptA = self.bank_bf()
                    ptB = self.bank_bf()
                    for j in range(4):
                        self.tr(ptA[:, j * 128:(j + 1) * 128], arT[:, j, 0, :], idb)
                    for j in range(4):
                        self.tr(ptA[:, 512 + j * 128:512 + (j + 1) * 128], v16[:, j, :], idb)
                    for j in range(4):
                        self.tr(ptB[:, j * 128:(j + 1) * 128], b16[:, j, :], idb)
                    for j in range(4):
                        self.tr(ptB[:, 512 + j * 128:512 + (j + 1) * 128], kp16[:, j, :], idb)
                    atok = atok_[par]
                    vtok = vtok_[par]
                    bhat = bhat_[par]
                    khat = khat_[par]
                    self.cp(atok[:], ptA[:, 0:512], eng="act")
                    self.cp(vtok[:], ptA[:, 512:1024], eng="act")
                    self.tt(bhat[:], ptB[:, 0:512], erev[:], ALU.mult)
                    self.tt(khat[:], ptB[:, 512:1024], erev[:], ALU.mult)
                    if self.cut == 7:
                        raise _Cut()
                    AT4 = AT4_[par]
                    PQ0 = PQ_[0]
                    for h in range(8):
                        j, q = h // 2, h % 2
                        rs = slice(q * 64, (q + 1) * 64)
                        pA = self.bank()
                        arv = arT[rs, j, :, :].rearrange("p w t -> p (w t)")
                        self.mm(pA[:, 0:256], btT[rs, j, :], arv)
                        self.mm(pA[:, 256:512], ktT[rs, j, :], arv)
                        self.tt(AT4[:, h, :], pA[:], mask4, ALU.mult)
                        if self.cut == 71 + h:
                            raise _Cut()
                    for q in range(2):
                        rs = slice(q * 64, (q + 1) * 64)
                        pq = self.bank()
                        pqv = pq[:].rearrange("p (a b) -> p a b", a=4)
                        for j in range(4):
                            self.mm(pqv[:, j, :], arT[rs, j, 0, :], btT[rs, j, :])
                        for j in range(4):
                            self.tt(PQ0[:, 2 * j + q, 1, :], pqv[:, j, :], maskQ, ALU.mult)
                    if self.cut == 8:
                        raise _Cut()
                    self.cp(PQ0[:, :, 0, :], AT4[:, :, 0:128], eng="pool")
                    TTa = TT_[0]
                    self.tt(TTa[:], AT4[:, :, 0:128], idb.unsqueeze(1).broadcast_to([128, 8, 128]), ALU.add, eng="pool")
                    cur = 0
                    tcur = 0
                    for lvl in range(1, 7):
                        PQc = PQ_[cur]
                        PQn = PQ_[1 - cur]
                        last = (lvl == 6)
                        for hb in range(4):
                            pb = self.bank()
                            pbv = pb[:].rearrange("p (h w t) -> p h w t", h=2, w=2)
                            for hh in range(2):
                                h = hb * 2 + hh
                                if not last:
                                    self.mm(pbv[:, hh, 0, :], PQc[:, h, 1, :], PQc[:, h, 0, :])
                                self.mm(pbv[:, hh, 1, :], PQc[:, h, 0, :], PQc[:, h, 1, :])
                            eng = "act" if hb % 2 == 0 else "dve"
                            if not last:
                                self.cp(PQn[:, hb * 2:hb * 2 + 2, :, :], pbv, eng=eng)
                            else:
                                self.cp(PQn[:, hb * 2:hb * 2 + 2, 1, :], pbv[:, :, 1, :], eng=eng)
                        TTc = TT_[tcur]
                        TTn = TT_[1 - tcur]
                        for hb in range(2):
                            pb = self.bank()
                            pbv = pb[:].rearrange("p (a b) -> p a b", a=4)
                            for hh in range(4):
                                h = hb * 4 + hh
                                self.mm(pbv[:, hh, :], PQn[:, h, 1, :], TTc[:, h, :], start=True, stop=False)
                                self.mm(pbv[:, hh, :], idb, TTc[:, h, :], start=False, stop=True)
                            self.cp(TTn[:, hb * 4:hb * 4 + 4, :], pbv, eng=("dve" if hb == 0 else "act"))
                        cur = 1 - cur
                        tcur = 1 - tcur
                    TT = TT_[tcur]
                    if self.cut == 9:
                        raise _Cut()
                    WT = WT_[par]
                    for hb in range(2):
                        pb = self.bank()
                        pbv = pb[:].rearrange("p (a b) -> p a b", a=4)
                        for hh in range(4):
                            h = hb * 4 + hh
                            j = h // 2
                            self.mm(pbv[:, hh, :], atok[:, j * 128:(j + 1) * 128], TT[:, h, :])
                        for q in range(2):
                            rs = slice(q * 64, (q + 1) * 64)
                            src = pb[rs, :].rearrange("p (j q t) -> p j q t", j=2, q=2)[:, :, q, :]
                            self.cp(WT[rs, hb * 2:hb * 2 + 2, :], src, eng=("act" if q == 0 else "dve"))
                    pav2 = self.bank()
                    for h in range(8):
                        self.mm(pav2[:, h * 64:(h + 1) * 64], AT4[:, h, 256:384], vtok[:, h * 64:(h + 1) * 64])
                    AV = AV_[par]
                    self.cp(AV[:], pav2[:], eng="act")
                    pu0 = self.bank()
                    for h in range(8):
                        self.mm(pu0[:, h * 64:(h + 1) * 64], TT[:, h, :], AV[:, h * 64:(h + 1) * 64])
                    U0 = U0_[par]
                    self.cp(U0[:], pu0[:], eng="act")
                    if self.cut == 10:
                        raise _Cut()
                    for q in range(2):
                        rs = slice(q * 64, (q + 1) * 64)
                        self.tt(H0bd[rs, :, q * 64:(q + 1) * 64], Hst[rs, :, :], bc3(gtm[rs, :, 1], 64), ALU.mult)
                    pX = self.bank()
                    for j in range(4):
                        self.mm(pX[:, j * 128:(j + 1) * 128], WT[:, j, :], H0bd[:, j, :])
                    U = U_[par]
                    self.tt(U[:], pX[:], U0[:], ALU.add)
                    pY = self.bank()
                    for h in range(8):
                        j, q = h // 2, h % 2
                        hs = slice(h * 64, (h + 1) * 64)
                        self.mm(pY[:, hs], arT[:, j, 1, :], H0bd[:, j, q * 64:(q + 1) * 64], start=True, stop=False)
                        self.mm(pY[:, hs], AT4[:, h, 128:256], U[:, hs], start=False, stop=False)
                        self.mm(pY[:, hs], AT4[:, h, 384:512], vtok[:, hs], start=False, stop=True)
                    pH = self.bank()
                    for j in range(4):
                        js = slice(j * 128, (j + 1) * 128)
                        self.mm(pH[:, js], bhat[:, js], U[:, js], start=True, stop=False)
                        self.mm(pH[:, js], khat[:, js], vtok[:, js], start=False, stop=True)
                    pHv = pH[:].rearrange("p (j c) -> p j c", j=4)
                    for q in range(2):
                        rs = slice(q * 64, (q + 1) * 64)
                        self.tt(Hst[rs, :, :], Hst[rs, :, :], bc3(gtm[rs, :, 0], 64), ALU.mult)
                        self.tt(Hst[rs, :, :], Hst[rs, :, :], pHv[rs, :, q * 64:(q + 1) * 64], ALU.add)
                    if self.cut == 11:
                        raise _Cut()
                    yv = yv_[par]
                    self.cp(yv[:], pY[:], eng="act")
                    y3 = yv[:].rearrange("p (h n) -> p h n", h=8)
                    self.red(st[:, 24:32], y3)
                    self.act(sq[:], yv[:], AF.Square)
                    self.red(st[:, 32:40], sq[:].rearrange("p (h n) -> p h n", h=8))
                    self.ts(st[:, 24:32], st[:, 24:32], 1.0 / 64, None, ALU.mult)
                    self.tt(st[:, 40:48], st[:, 24:32], st[:, 24:32], ALU.mult)
                    self.stt(st[:, 32:40], st[:, 32:40], 1.0 / 64, st[:, 40:48], ALU.mult, ALU.subtract)
                    self.act(st[:, 32:40], st[:, 32:40], AF.Sqrt, bias=GN_EPS)
                    self.recip(st[:, 32:40], st[:, 32:40])
                    self.tt(y3, y3, bc3(st[:, 24:32], 64), ALU.subtract)
                    self.tt(y3, y3, bc3(st[:, 32:40], 64), ALU.mult)
                    self.tt(yv[:], yv[:], lxw_bc, ALU.mult, eng="pool")
                    self.tt(yv[:], yv[:], lxb_bc, ALU.add, eng="pool")
                    pcf = self.bank()
                    for j in range(4):
                        self.mm(pcf[:, j * 2:(j + 1) * 2], rk[:, j, :], self.i2b[:])
                    coef = coef_[par]
                    self.cp(coef[:], pcf[:, 0:8])
                    self.tt(t3, vtok[:].rearrange("p (h n) -> p h n", h=8), bc3(coef[:], 64), ALU.mult)
                    self.tt(yv[:], yv[:], tA[:], ALU.add)
                    pg = self.bank()
                    self.mm(pg[:], sgT[:], g_up[:])
                    self.tt(ycat[:, 512:1024], yv[:], pg[:], ALU.mult)
                    if self.cut == 12:
                        raise _Cut()
                    pyT = self.bank_bf()
                    for c in range(8):
                        self.tr(pyT[:, c * 128:(c + 1) * 128], ycat[:, c * 128:(c + 1) * 128], idb)
                    yT = yT_[par]
                    self.cp(yT[:].rearrange("p c t -> p (c t)"), pyT[:], eng="act")
                    xo = xc
                    for half in range(2):
                        po = self.bank()
                        for c in range(8):
                            self.mm(po[:], yT[:, c, :], w_out[:, c, half * 512:(half + 1) * 512],
                                    start=(c == 0), stop=(c == 7))
                        self.tt(xo[:, half * 512:(half + 1) * 512], po[:], xc[:, half * 512:(half + 1) * 512], ALU.add)

                except _Cut:
                    xc = xc_[ch % 2]
                    xo = xc
                    par = ch % 2
                self.dma(xout[ch * 128:(ch + 1) * 128, :], xo[:], self.lane_o[par])

    def ffn_dense(self, xin, xout):
        nc = self.nc
        P = self.P
        TB = 2
        NT = NCH // TB
        NF = DFF // 128
        with ExitStack() as es:
            cnt = [0]

            def sb(shape, dt=F32, name=None):
                cnt[0] += 1
                return es.enter_context(nc.sbuf_tensor("f_%s%d" % (name or "t", cnt[0]), shape, dt))

            dbl = lambda shape, dt=F32, name=None: [sb(shape, dt, name), sb(shape, dt, name)]
            wg = sb([128, 8, DFF], BF16, "wg")
            wu = sb([128, 8, DFF], BF16, "wu")
            wd = sb([128, NF, D], BF16, "wd")
            cols = sb([128, KW], F32, "cols")
            lw = P.lane("fw", wait_all=True)
            lws = P.lane("fws", wait_all=True)
            self.dma(cols[:], self.cols_d[0], lws)
            for c in range(8):
                self.dma(wg[:, c, :], self.fg_d[0, c * 128:(c + 1) * 128, :], lw, eng="pool")
                self.dma(wu[:, c, :], self.fu_d[0, c * 128:(c + 1) * 128, :], lw, eng="pool")
            for c in range(0, NF, 2):
                self.dma(wd[:, c:c + 2, :], self.fd_d[0, c * 128:(c + 2) * 128, :].rearrange("(c p) n -> p c n", p=128),
                         lw, eng="pool")
            gcol = cols[:, K_GFFN:K_GFFN + 8]
            xt_ = dbl([128, TB, D], F32, "xt")
            xs_ = dbl([128, D], F32, "xs")
            ssq_ = dbl([128, 1], F32, "ssq")
            rstd_ = dbl([128, 1], F32, "rstd")
            hT_ = dbl([128, 8, TB * 128], BF16, "hT")
            sg_ = dbl([128, TB * 128], F32, "sg")
            hff_ = dbl([128, NF, TB * 128], BF16, "hff")
            lx = [P.lane("fx0"), P.lane("fx1")]
            lo = [P.lane("fo0"), P.lane("fo1")]
            W = TB * 128
            import os as _os
            for it in range(int(_os.environ.get('KFFN_NT', NT))):
                par = it % 2
                xt = xt_[par]
                hT = hT_[par]
                self.dma(xt[:], xin[it * W:(it + 1) * W, :].rearrange("(n p) d -> p n d", p=128), lx[par])
                for n in range(TB):
                    self.norm_T(xt[:, n, :], gcol, hT[:, :, n * 128:(n + 1) * 128],
                                {"xs": xs_[n % 2][:], "ssq": ssq_[n % 2][:], "rstd": rstd_[n % 2][:]})
                hff = hff_[par]
                for f in range(NF):
                    pg = self.bank()
                    pu = self.bank()
                    for c in range(8):
                        self.mm(pg[:, 0:W], wg[:, c, f * 128:(f + 1) * 128], hT[:, c, :], start=(c == 0), stop=(c == 7))
                    for c in range(8):
                        self.mm(pu[:, 0:W], wu[:, c, f * 128:(f + 1) * 128], hT[:, c, :], start=(c == 0), stop=(c == 7))
                    sg = sg_[f % 2]
                    self.act(sg[:], pg[:, 0:W], AF.Sigmoid)
                    self.tt(sg[:], sg[:], pg[:, 0:W], ALU.mult)
                    self.tt(hff[:, f, :], sg[:], pu[:, 0:W], ALU.mult)
                xo = xt
                for n in range(TB):
                    for half in range(2):
                        po = self.bank()
                        for f in range(NF):
                            self.mm(po[:], hff[:, f, n * 128:(n + 1) * 128], wd[:, f, half * 512:(half + 1) * 512],
                                    start=(f == 0), stop=(f == NF - 1))
                        self.tt(xo[:, n, half * 512:(half + 1) * 512], po[:], xt[:, n, half * 512:(half + 1) * 512],
                                ALU.add)
                self.dma(xout[it * W:(it + 1) * W, :].rearrange("(n p) d -> p n d", p=128), xo[:], lo[par])

    def moe(self, xin, yout):
        nc = self.nc
        P = self.P
        ST = 2048
        NB = ST // 128
        NP = DFE // 512
        with ExitStack() as es:
            cnt = [0]

            def sb(shape, dt=F32, name=None):
                cnt[0] += 1
                return es.enter_context(nc.sbuf_tensor("e_%s%d" % (name or "t", cnt[0]), shape, dt))

            dbl = lambda shape, dt=F32, name=None: [sb(shape, dt, name), sb(shape, dt, name)]
            cols = sb([128, KW], F32, "cols")
            rt32 = sb([128, 8, NE], F32, "rt32")
            rtg = sb([128, 8, 16], F32, "rtg")
            nfin = sb([128, 1, D], F32, "nfin")
            lw = P.lane("ew", wait_all=True)
            self.dma(cols[:], self.cols_d[1], lw)
            self.dma(rt32[:], self.rt_d, lw)
            self.dma(nfin[:], self.nfin_d.rearrange("(o d) -> o d", o=1).partition_broadcast(128), lw)
            gcol = cols[:, K_GFFN:K_GFFN + 8]
            self.tt(rtg[:, :, 0:NE], rt32[:], gcol.unsqueeze(2).broadcast_to([128, 8, NE]), ALU.mult)
            acc = sb([128, NB, D], F32, "acc")
            hT = sb([128, 8, ST], BF16, "hT")
            hT32_ = dbl([128, 8, 128], F32, "hT32")
            xs_ = dbl([128, D], F32, "xs")
            ssq_ = dbl([128, 1], F32, "ssq")
            rstd_ = dbl([128, 1], F32, "rstd")
            gates = sb([128, NB, NE], F32, "gates")
            sm = dbl([128, 64], F32, "sm")
            wgp_ = dbl([128, 8, 512], BF16, "wgp")
            wup_ = dbl([128, 8, 512], BF16, "wup")
            wdp_ = dbl([128, 4, D], BF16, "wdp")
            sg_ = dbl([128, 512], F32, "sg")
            hff_ = dbl([128, 4, 512], BF16, "hff")
            lxs = [P.lane("ex%d" % i, wait_all=True) for i in range(T // ST)]
            lwg = [[P.lane("ew%s%d" % (nm, i)) for nm in ("g", "u", "d")] for i in range(2)]
            lo = [P.lane("eo0"), P.lane("eo1")]
            import os as _os
            for s in range(T // ST):
                t0 = s * ST
                for n in range(NB):
                    self.dma(acc[:, n, :], xin[t0 + n * 128:t0 + (n + 1) * 128, :], lxs[s])
                for n in range(NB if int(_os.environ.get('KMCUT', '9')) >= 1 else 0):
                    par = n % 2
                    hT32 = hT32_[par]
                    self.norm_T(acc[:, n, :], gcol, hT[:, :, n * 128:(n + 1) * 128],
                                {"xs": xs_[par][:], "ssq": ssq_[par][:], "rstd": rstd_[par][:]}, hT32=hT32[:])
                    if int(_os.environ.get('KMCUT', '9')) < 2:
                        continue
                    pl = self.bank()
                    for c in range(8):
                        self.mm(pl[:, 0:NE], hT32[:, c, :], rtg[:, c, 0:NE], start=(c == 0), stop=(c == 7))
                    w = sm[par]
                    lg = w[:, 0:8]
                    self.cp(lg, pl[:, 0:NE])
                    self.red(w[:, 8:9], lg, op=ALU.max)
                    self.ts(w[:, 16:24], lg, w[:, 8:9], None, ALU.is_equal)
                    self.stt(w[:, 24:32], w[:, 16:24], -1e30, lg, ALU.mult, ALU.add)
                    self.red(w[:, 9:10], w[:, 24:32], op=ALU.max)
                    self.ts(w[:, 32:40], w[:, 24:32], w[:, 9:10], None, ALU.is_equal)
                    self.tt(w[:, 10:11], w[:, 9:10], w[:, 8:9], ALU.subtract)
                    self.act(w[:, 11:12], w[:, 10:11], AF.Exp)
                    self.ts(w[:, 12:13], w[:, 11:12], 1.0, None, ALU.add)
                    self.recip(w[:, 12:13], w[:, 12:13])
                    self.tt(w[:, 13:14], w[:, 11:12], w[:, 12:13], ALU.mult)
                    self.ts(w[:, 16:24], w[:, 16:24], w[:, 12:13], None, ALU.mult)
                    self.stt(gates[:, n, :], w[:, 32:40], w[:, 13:14], w[:, 16:24], ALU.mult, ALU.add)
                k = 0
                import os as _os
                for e in range(int(_os.environ.get('KMOE_E', NE)) if int(_os.environ.get('KMCUT', '9')) >= 3 else 0):
                    for pc in range(int(_os.environ.get('KMOE_P', NP))):
                        par = k % 2
                        k += 1
                        wgp, wup, wdp = wgp_[par], wup_[par], wdp_[par]
                        fs = slice(pc * 512, (pc + 1) * 512)
                        self.dma(wgp[:], self.mg_d[0, e, :, fs].rearrange("(c p) n -> p c n", p=128), lwg[par][0], eng="pool")
                        self.dma(wup[:], self.mu_d[0, e, :, fs].rearrange("(c p) n -> p c n", p=128), lwg[par][1], eng="pool")
                        self.dma(wdp[:], self.md_d[0, e, fs, :].rearrange("(c p) n -> p c n", p=128), lwg[par][2], eng="pool")
                        for tb in range(ST // 512):
                            hff = hff_[tb % 2]
                            ts_ = slice(tb * 512, (tb + 1) * 512)
                            for f in range(4):
                                pg = self.bank()
                                pu = self.bank()
                                for c in range(8):
                                    self.mm(pg[:], wgp[:, c, f * 128:(f + 1) * 128], hT[:, c, ts_],
                                            start=(c == 0), stop=(c == 7))
                                for c in range(8):
                                    self.mm(pu[:], wup[:, c, f * 128:(f + 1) * 128], hT[:, c, ts_],
                                            start=(c == 0), stop=(c == 7))
                                sg = sg_[f % 2]
                                self.act(sg[:], pg[:], AF.Sigmoid)
                                self.tt(sg[:], sg[:], pg[:], ALU.mult)
                                self.tt(hff[:, f, :], sg[:], pu[:], ALU.mult)
                            for nn in range(4):
                                n = tb * 4 + nn
                                for half in range(2):
                                    po = self.bank()
                                    for f in range(4):
                                        self.mm(po[:], hff[:, f, nn * 128:(nn + 1) * 128],
                                                wdp[:, f, half * 512:(half + 1) * 512], start=(f == 0), stop=(f == 3))
                                    a = acc[:, n, half * 512:(half + 1) * 512]
                                    self.stt(a, po[:], gates[:, n, e:e + 1], a, ALU.mult, ALU.add)
                for n in range(NB):
                    par = n % 2
                    xs = xs_[par]
                    if _os.environ.get('KMFIN', '1') == '0':
                        self.dma(yout[t0 + n * 128:t0 + (n + 1) * 128, :], acc[:, n, :], lo[par])
                        continue
                    self.act(xs[:], acc[:, n, :], AF.Square, accum_out=ssq_[par][:])
                    self.act(rstd_[par][:], ssq_[par][:], AF.Sqrt, scale=1.0 / D, bias=RMS_EPS)
                    self.recip(rstd_[par][:], rstd_[par][:])
                    self.stt(xs[:], acc[:, n, :], rstd_[par][:], nfin[:, 0, :], ALU.mult, ALU.mult)
                    self.dma(yout[t0 + n * 128:t0 + (n + 1) * 128, :], xs[:], lo[par])


_CACHE = {}


def _colfmt(v):
    v = np.asarray(v, np.float32)
    return np.ascontiguousarray(v.reshape(-1, 128).T)


def prep_shared(inp):
    f = lambda a: np.ascontiguousarray(np.asarray(a, np.float32))
    cols = np.zeros((2, 128, KW), np.float32)
    rows = np.zeros((2, 5, 512), np.float32)
    for l in range(2):
        cols[l, :, K_GMIX:K_GMIX + 8] = _colfmt(inp["norm_mix"][l])
        cols[l, :, K_GFFN:K_GFFN + 8] = _colfmt(inp["norm_ffn"][l])
        cols[l, :, K_MU:K_MU + 14] = _colfmt(inp["shift_mu"][l])
        cols[l, :, K_A0:K_A0 + 4] = _colfmt(inp["rw_a0"][l])
        cols[l, :, K_KK:K_KK + 4] = _colfmt(inp["rw_k_k"][l])
        cols[l, :, K_KA:K_KA + 4] = _colfmt(inp["rw_k_a"][l])
        cols[l, :, K_RK:K_RK + 4] = _colfmt(np.asarray(inp["rw_r_k"][l]).reshape(-1))
        rows[l, 0] = inp["rw_w0"][l]
        rows[l, 1] = inp["gm_ln_w"][l]
        rows[l, 2] = inp["gm_ln_b"][l]
        rows[l, 3] = inp["rw_lnx_w"][l]
        rows[l, 4] = inp["rw_lnx_b"][l]
    bsT = np.ascontiguousarray(np.transpose(np.asarray(inp["gm_bs"], np.float32), (0, 2, 1)))
    wsT = np.ascontiguousarray(np.transpose(np.asarray(inp["gm_ws"], np.float32), (0, 3, 1, 2)))
    shared = {
        "consts": make_consts(), "cols": cols, "rows": rows, "bsT": bsT, "wsT": wsT,
        "norm_final": f(inp["norm_final"]), "w_in": f(inp["w_in"]), "w_out": f(inp["w_out"]),
        "rw_w_up": f(inp["rw_w_up"]), "rw_a_up": f(inp["rw_a_up"]), "rw_g_up": f(inp["rw_g_up"]),
        "ffn_w_gate": f(inp["ffn_w_gate"]), "ffn_w_up": f(inp["ffn_w_up"]), "ffn_w_down": f(inp["ffn_w_down"]),
        "moe_router_l": np.ascontiguousarray(np.asarray(inp["moe_router"], np.float32)[0].reshape(8, 128, NE).transpose(1, 0, 2)), "moe_w_gate": f(inp["moe_w_gate"]), "moe_w_up": f(inp["moe_w_up"]),
        "moe_w_down": f(inp["moe_w_down"]),
    }
    return shared


def kernel(**inputs):
    x = np.asarray(inputs["x"], np.float32)
    shared = prep_shared(inputs)
    if "nc" not in _CACHE:
        _CACHE["nc"] = KB().build()
    nc = _CACHE["nc"]
    in_maps = []
    for i in range(8):
        m = dict(shared)
        m["x"] = np.ascontiguousarray(x[i])
        in_maps.append(m)
    res = run_bass_kernel_spmd(nc, in_maps, core_ids=list(range(8)))
    out = np.stack([np.asarray(r["y"], np.float32) for r in res.results], 0)
    return out
```

```python
import numpy as np
import concourse.bass as bass
import concourse.mybir as mybir
from concourse.bass_utils import run_bass_kernel_spmd
from contextlib import ExitStack

F32 = mybir.dt.float32
BF16 = mybir.dt.bfloat16
AF = mybir.ActivationFunctionType
ALU = mybir.AluOpType
AX = mybir.AxisListType

ENGS = ("pe", "act", "dve", "pool", "sp")


class Lane:
    def __init__(self, name, wait_all=False):
        self.name = name
        self.count = 0
        self.wait_all = wait_all
        self.last = None


class Op:
    __slots__ = ("eng", "emit", "deps", "lane", "lane_count", "signaled", "seq",
                 "eidx", "snap", "waits")


def _box(ap):
    t = ap.tensor
    esz = mybir.dt.size(t.dtype)
    name = t.name
    off = int(ap.offset)
    dims = [(int(s), int(c)) for (s, c) in ap.ap]
    if "DRam" in type(t).__name__:
        ext = 0
        for s, c in dims:
            ext += abs(s) * (c - 1)
        return (name, 0, 1, off * esz, (off + ext + 1) * esz)
    pstride = 1
    for d in list(t.shape)[1:]:
        pstride *= int(d)
    p0 = off // pstride
    f0 = off % pstride
    pext = 0
    fext = 0
    for i, (s, c) in enumerate(dims):
        if i == 0 and s != 0 and s % pstride == 0:
            pext += (s // pstride) * (c - 1)
        else:
            fext += abs(s) * (c - 1)
    if "PSum" in type(t).__name__:
        return (name, (p0 // 32) * 32, ((p0 + pext) // 32 + 1) * 32, 0, 2048)
    return (name, p0, p0 + pext + 1, f0 * esz, (f0 + fext + 1) * esz)


def _overlap(a, b):
    return a[1] < b[2] and b[1] < a[2] and a[3] < b[4] and b[3] < a[4]


def _covers(a, b):
    return a[1] <= b[1] and a[2] >= b[2] and a[3] <= b[3] and a[4] >= b[4]


class Prog:
    def __init__(self, nc):
        self.nc = nc
        self.ops = []
        self.eng_ops = {e: [] for e in ENGS}
        self.acc = {}
        self.lanes = []

    def lane(self, name, wait_all=False):
        l = Lane(name, wait_all)
        self.lanes.append(l)
        return l

    def add(self, eng, emit, reads=(), writes=(), lane=None, extra_deps=()):
        op = Op()
        op.eng = eng
        op.emit = emit
        op.lane = lane
        op.signaled = lane is not None
        op.seq = None
        op.snap = None
        op.waits = None
        if lane is not None:
            lane.count += 1
            op.lane_count = lane.count
            lane.last = op
        else:
            op.lane_count = 0
        deps = set(extra_deps)
        key = lane.name if lane is not None else eng
        rboxes = [_box(ap) for ap in reads]
        wboxes = [_box(ap) for ap in writes]
        wboxes += [b for b in rboxes if b[0].startswith("pb")]
        rboxes = [b for b in rboxes if not b[0].startswith("pb")]
        for b in rboxes:
            rec = self.acc.get(b[0])
            if rec is None:
                rec = self.acc[b[0]] = {"w": [], "r": []}
            for (wb, wop, wk) in rec["w"]:
                if _overlap(wb, b):
                    deps.add(wop)
        for b in wboxes:
            rec = self.acc.get(b[0])
            if rec is None:
                rec = self.acc[b[0]] = {"w": [], "r": []}
            for (wb, wop, wk) in rec["w"]:
                if _overlap(wb, b):
                    deps.add(wop)
            for (rb, rop, rk) in rec["r"]:
                if _overlap(rb, b):
                    deps.add(rop)
        for b in rboxes:
            rec = self.acc[b[0]]
            rec["r"] = [(rb, rop, rk) for (rb, rop, rk) in rec["r"]
                        if not (rk == key and _covers(b, rb))]
            rec["r"].append((b, op, key))
        for b in wboxes:
            rec = self.acc[b[0]]
            rec["w"] = [(wb, wop, wk) for (wb, wop, wk) in rec["w"] if not _covers(b, wb)]
            rec["r"] = [(rb, rop, rk) for (rb, rop, rk) in rec["r"] if not _covers(b, rb)]
            rec["w"].append((b, op, key))
        deps.discard(op)
        op.deps = deps
        op.eidx = len(self.eng_ops[eng])
        self.eng_ops[eng].append(op)
        self.ops.append(op)
        return op

    def barrier(self):
        lasts = []
        for e in ENGS:
            if self.eng_ops[e]:
                lasts.append(self.eng_ops[e][-1])
        for l in self.lanes:
            if l.last is not None:
                lasts.append(l.last)
        for e in ENGS:
            self.add(e, lambda eng: eng.nop(), extra_deps=lasts)
        self.acc = {}

    def _skip(self, op, d, win):
        if d.lane is not None:
            return False
        if d.eng != op.eng:
            return False
        if d.eng == "pe":
            return True
        if op.lane is None and op.eidx - d.eidx > win:
            return True
        return False

    def finalize(self, same_eng_window=3):
        for op in self.ops:
            for d in op.deps:
                if d.lane is None and not self._skip(op, d, same_eng_window):
                    d.signaled = True
        cnt = {e: 0 for e in ENGS}
        for op in self.ops:
            if op.lane is None and op.signaled:
                cnt[op.eng] += 1
                op.seq = cnt[op.eng]
        clock = {e: {} for e in ENGS}
        nwaits = 0
        for op in self.ops:
            ck = clock[op.eng]
            need = {}
            for d in op.deps:
                if self._skip(op, d, same_eng_window):
                    continue
                if d.lane is not None:
                    k = ("L", d.lane.name)
                    v = (d.lane.count if d.lane.wait_all else d.lane_count) * 16
                else:
                    k = ("E", d.eng)
                    v = d.seq
                if ck.get(k, 0) >= v:
                    continue
                if need.get(k, (0, None))[0] < v:
                    need[k] = (v, d)
            waits = []
            for k, (v, d) in need.items():
                if ck.get(k, 0) >= v:
                    continue
                waits.append((k, v))
                ck[k] = v
                if d.snap is not None:
                    for kk, vv in d.snap.items():
                        if ck.get(kk, 0) < vv:
                            ck[kk] = vv
            op.waits = waits
            nwaits += len(waits)
            if op.signaled:
                op.snap = dict(ck)
                if op.lane is None:
                    op.snap[("E", op.eng)] = max(op.snap.get(("E", op.eng), 0), op.seq)
        self.nwaits = nwaits

    def emit(self, es):
        nc = self.nc
        sems = {}
        for e in ENGS:
            sems[("E", e)] = es.enter_context(nc.semaphore("s_" + e))
        for l in self.lanes:
            if l.count > 0:
                sems[("L", l.name)] = es.enter_context(nc.semaphore("l_" + l.name))
        block = es.enter_context(nc.Block())

        def run(eng_name, e):
            for op in self.eng_ops[eng_name]:
                for (k, v) in op.waits:
                    e.wait_ge(sems[k], v)
                ins = op.emit(e)
                if op.lane is not None:
                    ins.then_inc(sems[("L", op.lane.name)], 16)
                elif op.signaled:
                    ins.then_inc(sems[("E", eng_name)], 1)

        @block.tensor
        def _(e):
            run("pe", e)

        @block.scalar
        def _(e):
            run("act", e)

        @block.vector
        def _(e):
            run("dve", e)

        @block.gpsimd
        def _(e):
            run("pool", e)

        @block.sync
        def _(e):
            run("sp", e)


T = 4096
D = 1024
NCH = T // 128
INW = 2816
DFF = 2816
NE = 8
DFE = 3584
CDEC = -float(np.exp(-0.5))
RMS_EPS = 1e-6
LN_EPS = 1e-5
GN_EPS = 64e-5

C_ID = 0
C_M4 = 128
C_MQ = 640
C_UT = 768
C_LT = 1024
C_BO = 1152
C_TM = 1280
C_I2 = 1282
CW = 1284
K_GMIX = 0
K_GFFN = 8
K_MU = 16
K_A0 = 30
K_KK = 34
K_KA = 38
K_RK = 42
KW = 46


def make_consts():
    c = np.zeros((128, CW), np.float32)
    s = np.arange(128)[:, None]
    t = np.arange(128)[None, :]
    c[:, C_ID:C_ID + 128] = (s == t)
    strict = (s < t).astype(np.float32)
    incl = (s <= t).astype(np.float32)
    c[:, C_M4:C_M4 + 512] = np.concatenate([strict, incl, strict, incl], 1)
    c[:, C_MQ:C_MQ + 128] = (t < s)
    mid = (s <= 63).astype(np.float32)
    c[:, C_UT:C_UT + 128] = CDEC * (incl - mid)
    c[:, C_UT + 128:C_UT + 256] = CDEC * (strict - mid)
    c[:, C_TM] = CDEC
    c[:, C_TM + 1] = CDEC * mid[:, 0]
    c[:, C_LT:C_LT + 128] = CDEC * (s > t)
    c[:, C_BO:C_BO + 128] = ((s // 64) == (t // 64))
    c[:, C_I2] = (s[:, 0] < 64)
    c[:, C_I2 + 1] = (s[:, 0] >= 64)
    return c


class _Cut(Exception):
    pass


class KB:
    def __init__(self, debug=False, stop_after=None, nch=NCH):
        self.nch = nch
        import os as _os
        self.cut = int(_os.environ.get('KCUT', '99'))
        self.debug = debug
        self.stop_after = stop_after
        self.nc = bass.Bass("TRN2", target_bir_lowering=False)
        self.P = Prog(self.nc)
        self.uid = 0

    def tt(self, out, in0, in1, op, eng="dve"):
        self.P.add(eng, lambda e: e.tensor_tensor(out=out, in0=in0, in1=in1, op=op), reads=[in0, in1], writes=[out])

    def ts(self, out, in0, s1, s2, op0, op1=None, eng="dve"):
        reads = [in0]
        if not isinstance(s1, (int, float)):
            reads.append(s1)
        if s2 is not None and not isinstance(s2, (int, float)):
            reads.append(s2)
        if op1 is None:
            self.P.add(eng, lambda e: e.tensor_scalar(out=out, in0=in0, scalar1=s1, scalar2=None, op0=op0),
                       reads=reads, writes=[out])
        else:
            self.P.add(eng, lambda e: e.tensor_scalar(out=out, in0=in0, scalar1=s1, scalar2=s2, op0=op0, op1=op1),
                       reads=reads, writes=[out])

    def stt(self, out, in0, scalar, in1, op0, op1):
        reads = [in0, in1]
        if not isinstance(scalar, (int, float)):
            reads.append(scalar)
        self.P.add("dve", lambda e: e.scalar_tensor_tensor(out=out, in0=in0, scalar=scalar, in1=in1, op0=op0, op1=op1),
                   reads=reads, writes=[out])

    def cp(self, out, in_, eng="dve"):
        if eng == "act":
            self.P.add("act", lambda e: e.copy(out=out, in_=in_), reads=[in_], writes=[out])
        else:
            self.P.add(eng, lambda e: e.tensor_copy(out=out, in_=in_), reads=[in_], writes=[out])

    def red(self, out, in_, op=ALU.add):
        self.P.add("dve", lambda e: e.tensor_reduce(out=out, in_=in_, axis=AX.X, op=op), reads=[in_], writes=[out])

    def recip(self, out, in_):
        self.P.add("dve", lambda e: e.reciprocal(out=out, in_=in_), reads=[in_], writes=[out])

    def act(self, out, in_, func, bias=None, scale=None, accum_out=None):
        reads = [in_]
        writes = [out]
        kw = {}
        if bias is not None:
            kw["bias"] = bias
            if not isinstance(bias, (int, float)):
                reads.append(bias)
        if scale is not None:
            kw["scale"] = scale
            if not isinstance(scale, (int, float)):
                reads.append(scale)
        if accum_out is not None:
            kw["accum_out"] = accum_out
            writes.append(accum_out)
        self.P.add("act", lambda e: e.activation(out=out, in_=in_, func=func, **kw), reads=reads, writes=writes)

    def mm(self, out, lhsT, rhs, start=True, stop=True):
        self.P.add("pe", lambda e: e.matmul(out, lhsT=lhsT, rhs=rhs, start=start, stop=stop),
                   reads=[lhsT, rhs], writes=[out])

    def tr(self, out, in_, ident):
        self.P.add("pe", lambda e: e.transpose(out=out, in_=in_, identity=ident), reads=[in_, ident], writes=[out])

    def dma(self, out, in_, lane, eng="sp"):
        self.P.add(eng, lambda e: e.dma_start(out=out, in_=in_), reads=[in_], writes=[out], lane=lane)

    def memset(self, ap, val, eng="pool"):
        self.P.add(eng, lambda e: e.memset(ap, val), writes=[ap])

    def bank(self):
        b = self.banks[self.bank_i % 8]
        self.bank_i += 1
        return b

    def bank_bf(self):
        return self.bank().bitcast(BF16)

    def build(self):
        nc = self.nc
        P = self.P
        dbg = self.debug
        din = lambda name, shape: nc.dram_tensor(name, shape, F32, kind="ExternalInput").ap()
        self.x = din("x", [T, D])
        self.consts_d = din("consts", [128, CW])
        self.cols_d = din("cols", [2, 128, KW])
        self.rows_d = din("rows", [2, 5, 512])
        self.bsT_d = din("bsT", [2, 128, 8])
        self.wsT_d = din("wsT", [2, 128, 8, 128])
        self.nfin_d = din("norm_final", [D])
        self.w_in_d = din("w_in", [2, D, INW])
        self.w_out_d = din("w_out", [2, D, D])
        self.w_up_d = din("rw_w_up", [2, 64, 512])
        self.a_up_d = din("rw_a_up", [2, 64, 512])
        self.g_up_d = din("rw_g_up", [2, 128, 512])
        self.fg_d = din("ffn_w_gate", [1, D, DFF])
        self.fu_d = din("ffn_w_up", [1, D, DFF])
        self.fd_d = din("ffn_w_down", [1, DFF, D])
        self.rt_d = din("moe_router_l", [128, 8, NE])
        self.mg_d = din("moe_w_gate", [1, NE, D, DFE])
        self.mu_d = din("moe_w_up", [1, NE, D, DFE])
        self.md_d = din("moe_w_down", [1, NE, DFE, D])
        self.y = nc.dram_tensor("y", [T, D], F32, kind="ExternalOutput").ap()
        skind = "ExternalOutput" if dbg else "Internal"
        self.xa = nc.dram_tensor("xa", [T, D], F32, kind=skind).ap()
        self.xb = nc.dram_tensor("xb", [T, D], F32, kind=skind).ap()
        self.xc = nc.dram_tensor("xc", [T, D], F32, kind=skind).ap()

        with ExitStack() as es:
            self.banks = [es.enter_context(nc.psum_tensor("pb%d" % i, [128, 512], F32)) for i in range(8)]
            self.bank_i = 0
            sb = lambda name, shape, dt: es.enter_context(nc.sbuf_tensor(name, shape, dt))
            self.cst = sb("cst", [128, CW], F32)
            self.idb = sb("idb", [128, 128], BF16)
            self.i2b = sb("i2b", [128, 2], BF16)
            self.lc = P.lane("const", wait_all=True)
            self.dma(self.cst[:], self.consts_d, self.lc)
            self.cp(self.idb[:], self.cst[:, C_ID:C_ID + 128])
            self.cp(self.i2b[:], self.cst[:, C_I2:C_I2 + 2])
            self.lane_x = [P.lane("x0"), P.lane("x1")]
            self.lane_o = [P.lane("o0"), P.lane("o1")]
            self.lane_w = P.lane("w", wait_all=False)

            stages = [("mix0", lambda: self.mixer(0, self.x, self.xa)),
                      ("ffn0", lambda: self.ffn_dense(self.xa, self.xb)),
                      ("mix1", lambda: self.mixer(1, self.xb, self.xc)),
                      ("moe", lambda: self.moe(self.xc, self.y))]
            import os as _os
            only = _os.environ.get("KSTAGES")
            for name, fn in stages:
                if only is not None and name not in only.split(","):
                    src, dst = {"mix0": (self.x, self.xa), "ffn0": (self.xa, self.xb), "mix1": (self.xb, self.xc),
                                "moe": (self.xc, self.y)}[name]
                    cp_lane = P.lane("cp_" + name, wait_all=True)
                    for ci in range(32):
                        self.dma(dst[ci * 128:(ci + 1) * 128, :], src[ci * 128:(ci + 1) * 128, :], cp_lane)
                else:
                    fn()
                P.barrier()
                if self.stop_after == name:
                    break
            outs = [self.y] + ([self.xa, self.xb, self.xc] if dbg else [])
            P.add("sp", lambda e: e.nop(), reads=outs)
            P.finalize()
            print("ops", len(P.ops), "waits", P.nwaits, {e: len(P.eng_ops[e]) for e in ENGS})
            P.emit(es)
        return nc

    def norm_T(self, xt, gcol, hT, tmp, hT32=None):
        xs, ssq, rstd = tmp["xs"], tmp["ssq"], tmp["rstd"]
        self.act(xs, xt, AF.Square, accum_out=ssq)
        self.act(rstd, ssq, AF.Sqrt, scale=1.0 / D, bias=RMS_EPS)
        self.recip(rstd, rstd)
        self.act(xs, xt, AF.Copy, scale=rstd)
        idf = self.cst[:, C_ID:C_ID + 128]
        for half in range(2):
            pb = self.bank()
            pv = pb[:].rearrange("p (a b) -> p a b", a=4)
            for c in range(4):
                cc = half * 4 + c
                self.tr(pv[:, c, :], xs[:, cc * 128:(cc + 1) * 128], idf)
            g = gcol[:, half * 4:(half + 1) * 4].unsqueeze(2).broadcast_to([128, 4, 128])
            self.tt(hT[:, half * 4:(half + 1) * 4, :], pv, g, ALU.mult)
            if hT32 is not None:
                self.cp(hT32[:, half * 4:(half + 1) * 4, :], pv, eng="act")

    def mixer(self, l, xin, xout):
        nc = self.nc
        P = self.P
        with ExitStack() as es:
            cnt = [0]

            def sb(shape, dt=F32, name=None):
                cnt[0] += 1
                return es.enter_context(nc.sbuf_tensor("m%d_%s%d" % (l, name or "t", cnt[0]), shape, dt))

            dbl = lambda shape, dt=F32, name=None: [sb(shape, dt, name), sb(shape, dt, name)]
            def sgl(shape, dt=F32, name=None):
                t_ = sb(shape, dt, name)
                return [t_, t_]
            w_in = sb([128, 8, INW], BF16, "win")
            w_out = sb([128, 8, D], BF16, "wout")
            w_up = sb([64, 512], BF16, "wup")
            a_up = sb([128, 512], BF16, "aup")
            g_up = sb([128, 512], BF16, "gup")
            cols = sb([128, KW], F32, "cols")
            omka = sb([128, 4], F32, "omka")
            rows = sb([128, 5, 512], F32, "rows")
            bsT = sb([128, 8], F32, "bsT")
            wsT32 = sb([128, 8, 128], F32, "wsT32")
            wsT = sb([128, 8, 128], BF16, "wsT")
            lw = P.lane("mw%d" % l, wait_all=True)
            lws = P.lane("mws%d" % l, wait_all=True)
            for c in range(8):
                self.dma(w_in[:, c, :], self.w_in_d[l, c * 128:(c + 1) * 128, :], lw, eng="pool")
            self.dma(w_out[:], self.w_out_d[l].rearrange("(c p) n -> p c n", p=128), lw, eng="pool")
            self.dma(w_up[:], self.w_up_d[l], lw, eng="pool")
            self.dma(a_up[64:128, :], self.a_up_d[l], lw, eng="pool")
            self.dma(g_up[:], self.g_up_d[l], lw, eng="pool")
            self.dma(cols[:], self.cols_d[l], lws)
            for i in range(5):
                self.dma(rows[:, i:i + 1, :], self.rows_d[l, i:i + 1, :].partition_broadcast(128), lws)
            self.dma(bsT[:], self.bsT_d[l], lws)
            self.dma(wsT32[:], self.wsT_d[l], lws)
            m_incl = self.cst[:, C_M4 + 128:C_M4 + 256]
            self.tt(wsT[:], wsT32[:], m_incl.unsqueeze(1).broadcast_to([128, 8, 128]), ALU.mult)
            self.ts(omka[:], cols[:, K_KA:K_KA + 4], -1.0, 1.0, ALU.mult, ALU.add)
            w0_bc = rows[:, 0, :]
            glw_bc = rows[:, 1, :]
            glb_bc = rows[:, 2, :]
            lxw_bc = rows[:, 3, :]
            lxb_bc = rows[:, 4, :]
            gcol = cols[:, K_GMIX:K_GMIX + 8]

            yv_holder = sgl([128, 512], F32, "yvtmp")
            xc_ = dbl([128, D], F32, "xc")
            xs_ = sgl([128, D], F32, "xs")
            ssq_ = sgl([128, 1], F32, "ssq")
            rstd_ = sgl([128, 1], F32, "rstd")
            hT_ = dbl([128, 8, 128], BF16, "hT")
            u_ = dbl([128, 512], F32, "u")
            vg_ = dbl([128, 512], F32, "vg")
            sq_ = sgl([128, 512], F32, "sq")
            st_ = sgl([128, 48], F32, "st")
            z_ = sgl([128, 512], BF16, "z")
            tA_ = sgl([128, 512], F32, "tA")
            ycat_ = sgl([128, D], BF16, "ycat")
            pf_ = dbl([128, 14, 129], F32, "pf")
            psf_ = sgl([128, 14, 128], F32, "psf")
            twT_ = sgl([64, 128], BF16, "twT")
            adT_ = sgl([128, 128], BF16, "adT")
            sgT_ = sgl([128, 128], BF16, "sgT")
            sigw_ = sgl([128, 512], F32, "sigw")
            gam_ = sgl([128, 4, 128], F32, "gam")
            igam_ = sgl([128, 4, 128], F32, "igam")
            gamx_ = sgl([128, 4, 128], F32, "gamx")
            erev_ = sgl([128, 512], F32, "erev")
            gtm_ = sgl([128, 4, 2], F32, "gtm")
            alr_ = sgl([128, 4, 128], F32, "alr")
            kkr_ = sgl([128, 512], F32, "kkr")
            kk_ = sgl([128, 512], F32, "kk")
            rn_ = sq_
            kp_ = sgl([128, 512], F32, "kp")
            bb_ = sgl([128, 512], F32, "bb")
            tmp_ = yv_holder
            arT_ = sgl([128, 4, 2, 128], BF16, "arT")
            ktT_ = sgl([128, 4, 128], BF16, "ktT")
            btT_ = sgl([128, 4, 128], BF16, "btT")
            rk_ = sgl([128, 4, 128], BF16, "rk")
            b16_ = sgl([128, 4, 128], BF16, "b16")
            kp16_ = sgl([128, 4, 128], BF16, "kp16")
            v16_ = sgl([128, 4, 128], BF16, "v16")
            atok_ = sgl([128, 512], BF16, "atok")
            vtok_ = sgl([128, 512], BF16, "vtok")
            bhat_ = sgl([128, 512], BF16, "bhat")
            khat_ = sgl([128, 512], BF16, "khat")
            AT4_ = sgl([128, 8, 512], BF16, "AT4")
            PQ_ = [sb([128, 8, 2, 128], BF16, "PQ") for _ in range(2)]
            TT_ = [sb([128, 8, 128], BF16, "TT") for _ in range(2)]
            WT_ = sgl([128, 4, 128], BF16, "WT")
            AV_ = sgl([128, 512], BF16, "AV")
            U0_ = sgl([128, 512], F32, "U0")
            U_ = sgl([128, 512], BF16, "U")
            H0bd = sb([128, 4, 128], BF16, "H0bd")
            Hst = sb([128, 4, 64], F32, "Hst")
            yv_ = yv_holder
            coef_ = sgl([128, 8], F32, "coef")
            yT_ = sgl([128, 8, 128], BF16, "yT")

            self.memset(H0bd[:], 0.0)
            self.memset(Hst[:], 0.0)
            self.memset(pf_[1][:, :, 128:129], 0.0)
            idb = self.idb[:]
            mask4 = self.cst[:, C_M4:C_M4 + 512]
            maskQ = self.cst[:, C_MQ:C_MQ + 128]
            UTall = self.cst[:, C_UT:C_UT + 256]
            TM = self.cst[:, C_TM:C_TM + 2]
            LT = self.cst[:, C_LT:C_LT + 128]
            BO = self.cst[:, C_BO:C_BO + 128]

            def bc3(ap2, n):
                a = ap2.shape[1]
                return ap2.unsqueeze(2).broadcast_to([ap2.shape[0], a, n])

            def stage_a(ch):
                par = ch % 2
                xc = xc_[par]
                st = st_[par]
                self.dma(xc[:], xin[ch * 128:(ch + 1) * 128, :], self.lane_x[par])
                hT = hT_[par]
                self.norm_T(xc[:], gcol, hT[:], {"xs": xs_[par][:], "ssq": ssq_[par][:], "rstd": rstd_[par][:]})
                pu = self.bank()
                pvb = self.bank()
                for c in range(8):
                    self.mm(pu[:], hT[:, c, :], w_in[:, c, 0:512], start=(c == 0), stop=(c == 7))
                for c in range(8):
                    self.mm(pvb[:], hT[:, c, :], w_in[:, c, 512:1024], start=(c == 0), stop=(c == 7))
                u = u_[par]
                vg = vg_[par]
                self.act(u[:], pu[:], AF.Gelu)
                self.act(vg[:], pvb[:], AF.Gelu)
                pf = pf_[par]
                psf = psf_[par]
                for jb in range(4):
                    nb = 4 if jb < 3 else 2
                    pb = self.bank()
                    pbv = pb[:].rearrange("p (a b) -> p a b", a=4)
                    for jj in range(nb):
                        j = jb * 4 + jj
                        for c in range(8):
                            self.mm(pbv[:, jj, :], w_in[:, c, 1024 + j * 128:1024 + (j + 1) * 128], hT[:, c, :],
                                    start=(c == 0), stop=(c == 7))
                    if jb % 2 == 0:
                        self.cp(pf[:, jb * 4:jb * 4 + nb, 1:129], pbv[:, 0:nb, :], eng="act")
                    else:
                        self.cp(pf[:, jb * 4:jb * 4 + nb, 1:129], pbv[:, 0:nb, :], eng="dve")
                self.cp(pf[:, :, 0:1], pf_[1 - par][:, :, 128:129], eng="pool")

            def stage_b(ch):
                par = ch % 2
                xc = xc_[par]
                st = st_[par]
                hT = hT_[par]
                u = u_[par]
                vg = vg_[par]
                pf = pf_[par]
                psf = psf_[par]
                try:
                    mu_bc = bc3(cols[:, K_MU:K_MU + 14], 128)
                    for (b0, b1, eg) in ((0, 8, "dve"), (8, 14, "pool")):
                        mb = bc3(cols[:, K_MU + b0:K_MU + b1], 128)
                        self.tt(psf[:, b0:b1, :], pf[:, b0:b1, 0:128], pf[:, b0:b1, 1:129], ALU.subtract, eng=eg)
                        self.tt(psf[:, b0:b1, :], psf[:, b0:b1, :], mb, ALU.mult, eng=eg)
                        self.tt(psf[:, b0:b1, :], psf[:, b0:b1, :], pf[:, b0:b1, 1:129], ALU.add, eng=eg)
                    rT = psf[:, 0:4, :]
                    kT = psf[:, 4:8, :]
                    vT = psf[:, 8:12, :]
                    if self.cut == 3:
                        raise _Cut()
                    sq = sq_[par]
                    self.red(st[:, 0:8], vg[:].rearrange("p (h n) -> p h n", h=8))
                    self.act(sq[:], vg[:], AF.Square)
                    self.red(st[:, 8:16], sq[:].rearrange("p (h n) -> p h n", h=8))
                    self.ts(st[:, 0:8], st[:, 0:8], 1.0 / 64, None, ALU.mult)
                    self.tt(st[:, 16:24], st[:, 0:8], st[:, 0:8], ALU.mult)
                    self.stt(st[:, 8:16], st[:, 8:16], 1.0 / 64, st[:, 16:24], ALU.mult, ALU.subtract)
                    self.act(st[:, 8:16], st[:, 8:16], AF.Sqrt, bias=LN_EPS)
                    self.recip(st[:, 8:16], st[:, 8:16])
                    tA = tA_[par]
                    v3 = vg[:].rearrange("p (h n) -> p h n", h=8)
                    t3 = tA[:].rearrange("p (h n) -> p h n", h=8)
                    self.tt(t3, v3, bc3(st[:, 0:8], 64), ALU.subtract)
                    self.tt(t3, t3, bc3(st[:, 8:16], 64), ALU.mult)
                    self.tt(tA[:], tA[:], glw_bc, ALU.mult, eng="pool")
                    z = z_[par]
                    self.tt(z[:], tA[:], glb_bc, ALU.add, eng="pool")
                    pm = self.bank()
                    for h in range(8):
                        self.mm(pm[:, h * 64:(h + 1) * 64], wsT[:, h, :], z[:, h * 64:(h + 1) * 64])
                    ycat = ycat_[par]
                    self.tt(t3, pm[:].rearrange("p (h n) -> p h n", h=8), bc3(bsT[:], 64), ALU.add)
                    self.tt(ycat[:, 0:512], tA[:], u[:], ALU.mult)
                    if self.cut == 4:
                        raise _Cut()
                    twT = twT_[par]
                    adT = adT_[par]
                    sgT = sgT_[par]
                    self.act(twT[:], psf[0:64, 12, :], AF.Tanh)
                    self.cp(adT[64:128, :], psf[64:128, 12, :], eng="pool")
                    self.act(sgT[:], psf[:, 13, :], AF.Sigmoid)
                    if self.cut == 41:
                        raise _Cut()
                    pd = self.bank()
                    self.mm(pd[:], twT[:], w_up[:])
                    sigw = sigw_[par]
                    self.tt(sigw[:], pd[:], w0_bc, ALU.add)
                    self.act(sigw[:], sigw[:], AF.Sigmoid)
                    if self.cut == 42:
                        raise _Cut()
                    pc0 = self.bank()
                    pc1 = self.bank()
                    pcs = [pc0, pc1]
                    for j in range(4):
                        self.mm(pcs[j // 2][:, (j % 2) * 256:(j % 2 + 1) * 256], sigw[:, j * 128:(j + 1) * 128], UTall)
                    if self.cut == 43:
                        raise _Cut()
                    ptm = self.bank()
                    for j in range(4):
                        self.mm(ptm[:, j * 2:(j + 1) * 2], sigw[:, j * 128:(j + 1) * 128], TM)
                    if self.cut == 44:
                        raise _Cut()
                    prv = self.bank()
                    self.mm(prv[:, 0:256], LT, sigw[:, 0:256])
                    self.mm(prv[:, 256:512], LT, sigw[:, 256:512])
                    if self.cut == 45:
                        raise _Cut()
                    gam = gam_[par]
                    igam = igam_[par]
                    gamx = gamx_[par]
                    erev = erev_[par]
                    gtm = gtm_[par]
                    for hb in range(2):
                        pcv = pcs[hb][:].rearrange("p (j w t) -> p j w t", j=2, w=2)
                        self.act(gam[:, hb * 2:(hb + 1) * 2, :], pcv[:, :, 0, :], AF.Exp)
                        self.act(igam[:, hb * 2:(hb + 1) * 2, :], pcv[:, :, 0, :], AF.Exp, scale=-1.0)
                        self.act(gamx[:, hb * 2:(hb + 1) * 2, :], pcv[:, :, 1, :], AF.Exp)
                    self.act(erev[:], prv[:], AF.Exp)
                    self.act(gtm[:].rearrange("p j w -> p (j w)"), ptm[:, 0:8], AF.Exp)
                    if self.cut == 5:
                        raise _Cut()
                    pa = self.bank()
                    pav = pa[:].rearrange("p (a b) -> p a b", a=4)
                    for j in range(4):
                        self.mm(pav[:, j, :], a_up[64:128, j * 128:(j + 1) * 128], adT[64:128, :])
                    alr = alr_[par]
                    for j in range(4):
                        self.act(alr[:, j, :], pav[:, j, :], AF.Sigmoid, bias=cols[:, K_A0 + j:K_A0 + j + 1])
                    kkr = kkr_[par]
                    kk = kk_[par]
                    rn = rn_[par]
                    kkr3 = kkr[:].rearrange("p (j t) -> p j t", j=4)
                    kk3 = kk[:].rearrange("p (j t) -> p j t", j=4)
                    self.tt(kkr3, kT, bc3(cols[:, K_KK:K_KK + 4], 128), ALU.mult)
                    self.tt(rn[:], kkr[:], kkr[:], ALU.mult)
                    pn = self.bank()
                    self.mm(pn[:, 0:256], BO, rn[:, 0:256])
                    self.mm(pn[:, 256:512], BO, rn[:, 256:512])
                    self.ts(rn[:], pn[:], 1e-18, None, ALU.max)
                    self.act(rn[:], rn[:], AF.Ln)
                    self.act(rn[:], rn[:], AF.Exp, scale=-0.5)
                    self.tt(kk[:], kkr[:], rn[:], ALU.mult)
                    tmp = tmp_[par]
                    tmp3 = tmp[:].rearrange("p (j t) -> p j t", j=4)
                    for j in range(4):
                        self.ts(tmp3[:, j, :], alr[:, j, :], cols[:, K_KA + j:K_KA + j + 1], omka[:, j:j + 1],
                                ALU.mult, ALU.add)
                    kp = kp_[par]
                    bb = bb_[par]
                    kp3 = kp[:].rearrange("p (j t) -> p j t", j=4)
                    bb3 = bb[:].rearrange("p (j t) -> p j t", j=4)
                    self.tt(kp3, kT, tmp3, ALU.mult)
                    self.tt(bb3, kk3, alr[:], ALU.mult, eng="pool")
                    arT = arT_[par]
                    ktT = ktT_[par]
                    btT = btT_[par]
                    rk = rk_[par]
                    self.tt(arT[:, :, 1, :], rT, gam[:], ALU.mult)
                    self.stt(arT[:, :, 0, :], kk3, -1.0, gamx[:], ALU.mult, ALU.mult)
                    self.tt(ktT[:], kp3, igam[:], ALU.mult)
                    self.tt(btT[:], bb3, igam[:], ALU.mult, eng="pool")
                    for j in range(4):
                        self.stt(rk[:, j, :], psf[:, j, :], cols[:, K_RK + j:K_RK + j + 1], kp3[:, j, :], ALU.mult, ALU.mult)
                    b16 = b16_[par]
                    kp16 = kp16_[par]
                    v16 = v16_[par]
                    self.cp(b16[:], bb3, eng="pool")
                    self.cp(kp16[:], kp3, eng="pool")
                    self.cp(v16[:], vT, eng="act")
                    if self.cut == 6:
                        raise _Cut()
                    ptA = self.bank_bf()
                    ptB = self.bank_bf()
                    for j in range(4):
                        self.tr(ptA[:, j * 128:(j + 1) * 128], arT[:, j, 0, :], idb)
                    for j in range(4):
                        self.tr(ptA[:, 512 + j * 128:512 + (j + 1) * 128], v16[:, j, :], idb)
                    for j in range(4):
                        self.tr(ptB[:, j * 128:(j + 1) * 128], b16[:, j, :], idb)
                    for j in range(4):
                        self.tr(ptB[:, 512 + j * 128:512 + (j + 1) * 128], kp16[:, j, :], idb)
                    atok = atok_[par]
                    vtok = vtok_[par]
                    bhat = bhat_[par]
                    khat = khat_[par]
                    self.cp(atok[:], ptA[:, 0:512], eng="act")
                    self.cp(vtok[:], ptA[:, 512:1024], eng="act")
                    self.tt(bhat[:], ptB[:, 0:512], erev[:], ALU.mult)
                    self.tt(khat[:], ptB[:, 512:1024], erev[:], ALU.mult)
                    if self.cut == 7:
                        raise _Cut()
                    AT4 = AT4_[par]
                    PQ0 = PQ_[0]
                    for h in range(8):
                        j, q = h // 2, h % 2
                        rs = slice(q * 64, (q + 1) * 64)
                        pA = self.bank()
                        arv = arT[rs, j, :, :].rearrange("p w t -> p (w t)")
                        self.mm(pA[:, 0:256], btT[rs, j, :], arv)
                        self.mm(pA[:, 256:512], ktT[rs, j, :], arv)
                        self.tt(AT4[:, h, :], pA[:], mask4, ALU.mult)
                        if self.cut == 71 + h:
                            raise _Cut()
                    for q in range(2):
                        rs = slice(q * 64, (q + 1) * 64)
                        pq = self.bank()
                        pqv = pq[:].rearrange("p (a b) -> p a b", a=4)
                        for j in range(4):
                            self.mm(pqv[:, j, :], arT[rs, j, 0, :], btT[rs, j, :])
                        for j in range(4):
                            self.tt(PQ0[:, 2 * j + q, 1, :], pqv[:, j, :], maskQ, ALU.mult)
                    if self.cut == 8:
                        raise _Cut()
                    self.cp(PQ0[:, :, 0, :], AT4[:, :, 0:128], eng="pool")
                    TTa = TT_[0]
                    self.tt(TTa[:], AT4[:, :, 0:128], idb.unsqueeze(1).broadcast_to([128, 8, 128]), ALU.add, eng="pool")
                    cur = 0
                    tcur = 0
                    for lvl in range(1, 7):
                        PQc = PQ_[cur]
                        PQn = PQ_[1 - cur]
                        last = (lvl == 6)
                        for hb in range(4):
                            pb = self.bank()
                            pbv = pb[:].rearrange("p (h w t) -> p h w t", h=2, w=2)
                            for hh in range(2):
                                h = hb * 2 + hh
                                if not last:
                                    self.mm(pbv[:, hh, 0, :], PQc[:, h, 1, :], PQc[:, h, 0, :])
                                self.mm(pbv[:, hh, 1, :], PQc[:, h, 0, :], PQc[:, h, 1, :])
                            eng = "act" if hb % 2 == 0 else "dve"
                            if not last:
                                self.cp(PQn[:, hb * 2:hb * 2 + 2, :, :], pbv, eng=eng)
                            else:
                                self.cp(PQn[:, hb * 2:hb * 2 + 2, 1, :], pbv[:, :, 1, :], eng=eng)
                        TTc = TT_[tcur]
                        TTn = TT_[1 - tcur]
                        for hb in range(2):
                            pb = self.bank()
                            pbv = pb[:].rearrange("p (a b) -> p a b", a=4)
                            for hh in range(4):
                                h = hb * 4 + hh
                                self.mm(pbv[:, hh, :], PQn[:, h, 1, :], TTc[:, h, :], start=True, stop=False)
                                self.mm(pbv[:, hh, :], idb, TTc[:, h, :], start=False, stop=True)
                            self.cp(TTn[:, hb * 4:hb * 4 + 4, :], pbv, eng=("dve" if hb == 0 else "act"))
                        cur = 1 - cur
                        tcur = 1 - tcur
                    TT = TT_[tcur]
                    if self.cut == 9:
                        raise _Cut()
                    WT = WT_[par]
                    for hb in range(2):
                        pb = self.bank()
                        pbv = pb[:].rearrange("p (a b) -> p a b", a=4)
                        for hh in range(4):
                            h = hb * 4 + hh
                            j = h // 2
                            self.mm(pbv[:, hh, :], atok[:, j * 128:(j + 1) * 128], TT[:, h, :])
                        for q in range(2):
                            rs = slice(q * 64, (q + 1) * 64)
                            src = pb[rs, :].rearrange("p (j q t) -> p j q t", j=2, q=2)[:, :, q, :]
                            self.cp(WT[rs, hb * 2:hb * 2 + 2, :], src, eng=("act" if q == 0 else "dve"))
                    pav2 = self.bank()
                    for h in range(8):
                        self.mm(pav2[:, h * 64:(h + 1) * 64], AT4[:, h, 256:384], vtok[:, h * 64:(h + 1) * 64])
                    AV = AV_[par]
                    self.cp(AV[:], pav2[:], eng="act")
                    pu0 = self.bank()
                    for h in range(8):
                        self.mm(pu0[:, h * 64:(h + 1) * 64], TT[:, h, :], AV[:, h * 64:(h + 1) * 64])
                    U0 = U0_[par]
                    self.cp(U0[:], pu0[:], eng="act")
                    if self.cut == 10:
                        raise _Cut()
                    for q in range(2):
                        rs = slice(q * 64, (q + 1) * 64)
                        self.tt(H0bd[rs, :, q * 64:(q + 1) * 64], Hst[rs, :, :], bc3(gtm[rs, :, 1], 64), ALU.mult)
                    pX = self.bank()
                    for j in range(4):
                        self.mm(pX[:, j * 128:(j + 1) * 128], WT[:, j, :], H0bd[:, j, :])
                    U = U_[par]
                    self.tt(U[:], pX[:], U0[:], ALU.add)
                    pY = self.bank()
                    for h in range(8):
                        j, q = h // 2, h % 2
                        hs = slice(h * 64, (h + 1) * 64)
                        self.mm(pY[:, hs], arT[:, j, 1, :], H0bd[:, j, q * 64:(q + 1) * 64], start=True, stop=False)
                        self.mm(pY[:, hs], AT4[:, h, 128:256], U[:, hs], start=False, stop=False)
                        self.mm(pY[:, hs], AT4[:, h, 384:512], vtok[:, hs], start=False, stop=True)
                    pH = self.bank()
                    for j in range(4):
                        js = slice(j * 128, (j + 1) * 128)
                        self.mm(pH[:, js], bhat[:, js], U[:, js], start=True, stop=False)
                        self.mm(pH[:, js], khat[:, js], vtok[:, js], start=False, stop=True)
                    pHv = pH[:].rearrange("p (j c) -> p j c", j=4)
                    for q in range(2):
                        rs = slice(q * 64, (q + 1) * 64)
                        self.tt(Hst[rs, :, :], Hst[rs, :, :], bc3(gtm[rs, :, 0], 64), ALU.mult)
                        self.tt(Hst[rs, :, :], Hst[rs, :, :], pHv[rs, :, q * 64:(q + 1) * 64], ALU.add)
                    if self.cut == 11:
                        raise _Cut()
                    yv = yv_[par]
                    self.cp(yv[:], pY[:], eng="act")
                    y3 = yv[:].rearrange("p (h n) -> p h n", h=8)
                    self.red(st[:, 24:32], y3)
                    self.act(sq[:], yv[:], AF.Square)
                    self.red(st[:, 32:40], sq[:].rearrange("p (h n) -> p h n", h=8))
                    self.ts(st[:, 24:32], st[:, 24:32], 1.0 / 64, None, ALU.mult)
                    self.tt(st[:, 40:48], st[:, 24:32], st[:, 24:32], ALU.mult)
                    self.stt(st[:, 32:40], st[:, 32:40], 1.0 / 64, st[:, 40:48], ALU.mult, ALU.subtract)
                    self.act(st[:, 32:40], st[:, 32:40], AF.Sqrt, bias=GN_EPS)
                    self.recip(st[:, 32:40], st[:, 32:40])
                    self.tt(y3, y3, bc3(st[:, 24:32], 64), ALU.subtract)
                    self.tt(y3, y3, bc3(st[:, 32:40], 64), ALU.mult)
                    self.tt(yv[:], yv[:], lxw_bc, ALU.mult, eng="pool")
                    self.tt(yv[:], yv[:], lxb_bc, ALU.add, eng="pool")
                    pcf = self.bank()
                    for j in range(4):
                        self.mm(pcf[:, j * 2:(j + 1) * 2], rk[:, j, :], self.i2b[:])
                    coef = coef_[par]
                    self.cp(coef[:], pcf[:, 0:8])
                    self.tt(t3, vtok[:].rearrange("p (h n) -> p h n", h=8), bc3(coef[:], 64), ALU.mult)
                    self.tt(yv[:], yv[:], tA[:], ALU.add)
                    pg = self.bank()
                    self.mm(pg[:], sgT[:], g_up[:])
                    self.tt(ycat[:, 512:1024], yv[:], pg[:], ALU.mult)
                    if self.cut == 12:
                        raise _Cut()
                    pyT = self.bank_bf()
                    for c in range(8):
                        self.tr(pyT[:, c * 128:(c + 1) * 128], ycat[:, c * 128:(c + 1) * 128], idb)
                    yT = yT_[par]
                    self.cp(yT[:].rearrange("p c t -> p (c t)"), pyT[:], eng="act")
                    xo = xc
                    for half in range(2):
                        po = self.bank()
                        for c in range(8):
                            self.mm(po[:], yT[:, c, :], w_out[:, c, half * 512:(half + 1) * 512],
                                    start=(c == 0), stop=(c == 7))
                        self.tt(xo[:, half * 512:(half + 1) * 512], po[:], xc[:, half * 512:(half + 1) * 512], ALU.add)

                except _Cut:
                    xo = xc
                self.dma(xout[ch * 128:(ch + 1) * 128, :], xo[:], self.lane_o[par])

            if self.nch > 0:
                stage_a(0)
            for ch in range(self.nch):
                if ch + 1 < self.nch:
                    stage_a(ch + 1)
                stage_b(ch)

    def ffn_dense(self, xin, xout):
        nc = self.nc
        P = self.P
        TB = 2
        NT = NCH // TB
        NF = DFF // 128
        with ExitStack() as es:
            cnt = [0]

            def sb(shape, dt=F32, name=None):
                cnt[0] += 1
                return es.enter_context(nc.sbuf_tensor("f_%s%d" % (name or "t", cnt[0]), shape, dt))

            dbl = lambda shape, dt=F32, name=None: [sb(shape, dt, name), sb(shape, dt, name)]
            wg = sb([128, 8, DFF], BF16, "wg")
            wu = sb([128, 8, DFF], BF16, "wu")
            wd = sb([128, NF, D], BF16, "wd")
            cols = sb([128, KW], F32, "cols")
            lw = P.lane("fw", wait_all=True)
            lws = P.lane("fws", wait_all=True)
            self.dma(cols[:], self.cols_d[0], lws)
            for c in range(8):
                self.dma(wg[:, c, :], self.fg_d[0, c * 128:(c + 1) * 128, :], lw, eng="pool")
                self.dma(wu[:, c, :], self.fu_d[0, c * 128:(c + 1) * 128, :], lw, eng="pool")
            for c in range(0, NF, 2):
                self.dma(wd[:, c:c + 2, :], self.fd_d[0, c * 128:(c + 2) * 128, :].rearrange("(c p) n -> p c n", p=128),
                         lw, eng="pool")
            gcol = cols[:, K_GFFN:K_GFFN + 8]
            xt_ = dbl([128, TB, D], F32, "xt")
            xs_ = dbl([128, D], F32, "xs")
            ssq_ = dbl([128, 1], F32, "ssq")
            rstd_ = dbl([128, 1], F32, "rstd")
            hT_ = dbl([128, 8, TB * 128], BF16, "hT")
            sg_ = dbl([128, TB * 128], F32, "sg")
            hff_ = dbl([128, NF, TB * 128], BF16, "hff")
            lx = [P.lane("fx0"), P.lane("fx1")]
            lo = [P.lane("fo0"), P.lane("fo1")]
            W = TB * 128
            import os as _os
            for it in range(int(_os.environ.get('KFFN_NT', NT))):
                par = it % 2
                xt = xt_[par]
                hT = hT_[par]
                self.dma(xt[:], xin[it * W:(it + 1) * W, :].rearrange("(n p) d -> p n d", p=128), lx[par])
                for n in range(TB):
                    self.norm_T(xt[:, n, :], gcol, hT[:, :, n * 128:(n + 1) * 128],
                                {"xs": xs_[n % 2][:], "ssq": ssq_[n % 2][:], "rstd": rstd_[n % 2][:]})
                hff = hff_[par]
                for f in range(NF):
                    pg = self.bank()
                    pu = self.bank()
                    for c in range(8):
                        self.mm(pg[:, 0:W], wg[:, c, f * 128:(f + 1) * 128], hT[:, c, :], start=(c == 0), stop=(c == 7))
                    for c in range(8):
                        self.mm(pu[:, 0:W], wu[:, c, f * 128:(f + 1) * 128], hT[:, c, :], start=(c == 0), stop=(c == 7))
                    sg = sg_[f % 2]
                    self.act(sg[:], pg[:, 0:W], AF.Sigmoid)
                    self.tt(sg[:], sg[:], pg[:, 0:W], ALU.mult)
                    self.tt(hff[:, f, :], sg[:], pu[:, 0:W], ALU.mult)
                xo = xt
                for n in range(TB):
                    for half in range(2):
                        po = self.bank()
                        for f in range(NF):
                            self.mm(po[:], hff[:, f, n * 128:(n + 1) * 128], wd[:, f, half * 512:(half + 1) * 512],
                                    start=(f == 0), stop=(f == NF - 1))
                        self.tt(xo[:, n, half * 512:(half + 1) * 512], po[:], xt[:, n, half * 512:(half + 1) * 512],
                                ALU.add)
                self.dma(xout[it * W:(it + 1) * W, :].rearrange("(n p) d -> p n d", p=128), xo[:], lo[par])

    def moe(self, xin, yout):
        nc = self.nc
        P = self.P
        ST = 2048
        NB = ST // 128
        NP = DFE // 512
        with ExitStack() as es:
            cnt = [0]

            def sb(shape, dt=F32, name=None):
                cnt[0] += 1
                return es.enter_context(nc.sbuf_tensor("e_%s%d" % (name or "t", cnt[0]), shape, dt))

            dbl = lambda shape, dt=F32, name=None: [sb(shape, dt, name), sb(shape, dt, name)]
            cols = sb([128, KW], F32, "cols")
            rt32 = sb([128, 8, NE], F32, "rt32")
            rtg = sb([128, 8, 16], F32, "rtg")
            nfin = sb([128, 1, D], F32, "nfin")
            lw = P.lane("ew", wait_all=True)
            self.dma(cols[:], self.cols_d[1], lw)
            self.dma(rt32[:], self.rt_d, lw)
            self.dma(nfin[:], self.nfin_d.rearrange("(o d) -> o d", o=1).partition_broadcast(128), lw)
            gcol = cols[:, K_GFFN:K_GFFN + 8]
            self.tt(rtg[:, :, 0:NE], rt32[:], gcol.unsqueeze(2).broadcast_to([128, 8, NE]), ALU.mult)
            acc = sb([128, NB, D], F32, "acc")
            hT = sb([128, 8, ST], BF16, "hT")
            hT32_ = dbl([128, 8, 128], F32, "hT32")
            xs_ = dbl([128, D], F32, "xs")
            ssq_ = dbl([128, 1], F32, "ssq")
            rstd_ = dbl([128, 1], F32, "rstd")
            gates = sb([128, NB, NE], F32, "gates")
            sm = dbl([128, 64], F32, "sm")
            wgp_ = dbl([128, 8, 512], BF16, "wgp")
            wup_ = dbl([128, 8, 512], BF16, "wup")
            wdp_ = dbl([128, 4, D], BF16, "wdp")
            sg_ = dbl([128, 512], F32, "sg")
            hff_ = dbl([128, 4, 512], BF16, "hff")
            lxs = [P.lane("ex%d" % i, wait_all=True) for i in range(T // ST)]
            lwg = [[P.lane("ew%s%d" % (nm, i)) for nm in ("g", "u", "d")] for i in range(2)]
            lo = [P.lane("eo0"), P.lane("eo1")]
            import os as _os
            for s in range(T // ST):
                t0 = s * ST
                for n in range(NB):
                    self.dma(acc[:, n, :], xin[t0 + n * 128:t0 + (n + 1) * 128, :], lxs[s])
                for n in range(NB if int(_os.environ.get('KMCUT', '9')) >= 1 else 0):
                    par = n % 2
                    hT32 = hT32_[par]
                    self.norm_T(acc[:, n, :], gcol, hT[:, :, n * 128:(n + 1) * 128],
                                {"xs": xs_[par][:], "ssq": ssq_[par][:], "rstd": rstd_[par][:]}, hT32=hT32[:])
                    if int(_os.environ.get('KMCUT', '9')) < 2:
                        continue
                    pl = self.bank()
                    for c in range(8):
                        self.mm(pl[:, 0:NE], hT32[:, c, :], rtg[:, c, 0:NE], start=(c == 0), stop=(c == 7))
                    w = sm[par]
                    lg = w[:, 0:8]
                    self.cp(lg, pl[:, 0:NE])
                    self.red(w[:, 8:9], lg, op=ALU.max)
                    self.ts(w[:, 16:24], lg, w[:, 8:9], None, ALU.is_equal)
                    self.stt(w[:, 24:32], w[:, 16:24], -1e30, lg, ALU.mult, ALU.add)
                    self.red(w[:, 9:10], w[:, 24:32], op=ALU.max)
                    self.ts(w[:, 32:40], w[:, 24:32], w[:, 9:10], None, ALU.is_equal)
                    self.tt(w[:, 10:11], w[:, 9:10], w[:, 8:9], ALU.subtract)
                    self.act(w[:, 11:12], w[:, 10:11], AF.Exp)
                    self.ts(w[:, 12:13], w[:, 11:12], 1.0, None, ALU.add)
                    self.recip(w[:, 12:13], w[:, 12:13])
                    self.tt(w[:, 13:14], w[:, 11:12], w[:, 12:13], ALU.mult)
                    self.ts(w[:, 16:24], w[:, 16:24], w[:, 12:13], None, ALU.mult)
                    self.stt(gates[:, n, :], w[:, 32:40], w[:, 13:14], w[:, 16:24], ALU.mult, ALU.add)
                k = 0
                import os as _os
                for e in range(int(_os.environ.get('KMOE_E', NE)) if int(_os.environ.get('KMCUT', '9')) >= 3 else 0):
                    for pc in range(int(_os.environ.get('KMOE_P', NP))):
                        par = k % 2
                        k += 1
                        wgp, wup, wdp = wgp_[par], wup_[par], wdp_[par]
                        fs = slice(pc * 512, (pc + 1) * 512)
                        self.dma(wgp[:], self.mg_d[0, e, :, fs].rearrange("(c p) n -> p c n", p=128), lwg[par][0], eng="pool")
                        self.dma(wup[:], self.mu_d[0, e, :, fs].rearrange("(c p) n -> p c n", p=128), lwg[par][1], eng="pool")
                        self.dma(wdp[:], self.md_d[0, e, fs, :].rearrange("(c p) n -> p c n", p=128), lwg[par][2], eng="pool")
                        for tb in range(ST // 512):
                            hff = hff_[tb % 2]
                            ts_ = slice(tb * 512, (tb + 1) * 512)
                            for f in range(4):
                                pg = self.bank()
                                pu = self.bank()
                                for c in range(8):
                                    self.mm(pg[:], wgp[:, c, f * 128:(f + 1) * 128], hT[:, c, ts_],
                                            start=(c == 0), stop=(c == 7))
                                for c in range(8):
                                    self.mm(pu[:], wup[:, c, f * 128:(f + 1) * 128], hT[:, c, ts_],
                                            start=(c == 0), stop=(c == 7))
                                sg = sg_[f % 2]
                                self.act(sg[:], pg[:], AF.Sigmoid)
                                self.tt(sg[:], sg[:], pg[:], ALU.mult)
                                self.tt(hff[:, f, :], sg[:], pu[:], ALU.mult)
                            for nn in range(4):
                                n = tb * 4 + nn
                                for half in range(2):
                                    po = self.bank()
                                    for f in range(4):
                                        self.mm(po[:], hff[:, f, nn * 128:(nn + 1) * 128],
                                                wdp[:, f, half * 512:(half + 1) * 512], start=(f == 0), stop=(f == 3))
                                    a = acc[:, n, half * 512:(half + 1) * 512]
                                    self.stt(a, po[:], gates[:, n, e:e + 1], a, ALU.mult, ALU.add)
                for n in range(NB):
                    par = n % 2
                    xs = xs_[par]
                    if _os.environ.get('KMFIN', '1') == '0':
                        self.dma(yout[t0 + n * 128:t0 + (n + 1) * 128, :], acc[:, n, :], lo[par])
                        continue
                    self.act(xs[:], acc[:, n, :], AF.Square, accum_out=ssq_[par][:])
                    self.act(rstd_[par][:], ssq_[par][:], AF.Sqrt, scale=1.0 / D, bias=RMS_EPS)
                    self.recip(rstd_[par][:], rstd_[par][:])
                    self.stt(xs[:], acc[:, n, :], rstd_[par][:], nfin[:, 0, :], ALU.mult, ALU.mult)
                    self.dma(yout[t0 + n * 128:t0 + (n + 1) * 128, :], xs[:], lo[par])


_CACHE = {}


def _colfmt(v):
    v = np.asarray(v, np.float32)
    return np.ascontiguousarray(v.reshape(-1, 128).T)


def prep_shared(inp):
    f = lambda a: np.ascontiguousarray(np.asarray(a, np.float32))
    cols = np.zeros((2, 128, KW), np.float32)
    rows = np.zeros((2, 5, 512), np.float32)
    for l in range(2):
        cols[l, :, K_GMIX:K_GMIX + 8] = _colfmt(inp["norm_mix"][l])
        cols[l, :, K_GFFN:K_GFFN + 8] = _colfmt(inp["norm_ffn"][l])
        cols[l, :, K_MU:K_MU + 14] = _colfmt(inp["shift_mu"][l])
        cols[l, :, K_A0:K_A0 + 4] = _colfmt(inp["rw_a0"][l])
        cols[l, :, K_KK:K_KK + 4] = _colfmt(inp["rw_k_k"][l])
        cols[l, :, K_KA:K_KA + 4] = _colfmt(inp["rw_k_a"][l])
        cols[l, :, K_RK:K_RK + 4] = _colfmt(np.asarray(inp["rw_r_k"][l]).reshape(-1))
        rows[l, 0] = inp["rw_w0"][l]
        rows[l, 1] = inp["gm_ln_w"][l]
        rows[l, 2] = inp["gm_ln_b"][l]
        rows[l, 3] = inp["rw_lnx_w"][l]
        rows[l, 4] = inp["rw_lnx_b"][l]
    bsT = np.ascontiguousarray(np.transpose(np.asarray(inp["gm_bs"], np.float32), (0, 2, 1)))
    wsT = np.ascontiguousarray(np.transpose(np.asarray(inp["gm_ws"], np.float32), (0, 3, 1, 2)))
    shared = {
        "consts": make_consts(), "cols": cols, "rows": rows, "bsT": bsT, "wsT": wsT,
        "norm_final": f(inp["norm_final"]), "w_in": f(inp["w_in"]), "w_out": f(inp["w_out"]),
        "rw_w_up": f(inp["rw_w_up"]), "rw_a_up": f(inp["rw_a_up"]), "rw_g_up": f(inp["rw_g_up"]),
        "ffn_w_gate": f(inp["ffn_w_gate"]), "ffn_w_up": f(inp["ffn_w_up"]), "ffn_w_down": f(inp["ffn_w_down"]),
        "moe_router_l": np.ascontiguousarray(np.asarray(inp["moe_router"], np.float32)[0].reshape(8, 128, NE).transpose(1, 0, 2)), "moe_w_gate": f(inp["moe_w_gate"]), "moe_w_up": f(inp["moe_w_up"]),
        "moe_w_down": f(inp["moe_w_down"]),
    }
    return shared


def kernel(**inputs):
    x = np.asarray(inputs["x"], np.float32)
    shared = prep_shared(inputs)
    if "nc" not in _CACHE:
        _CACHE["nc"] = KB().build()
    nc = _CACHE["nc"]
    in_maps = []
    for i in range(8):
        m = dict(shared)
        m["x"] = np.ascontiguousarray(x[i])
        in_maps.append(m)
    res = run_bass_kernel_spmd(nc, in_maps, core_ids=list(range(8)))
    out = np.stack([np.asarray(r["y"], np.float32) for r in res.results], 0)
    return out
```
